# Optimizing a Trainium2 kernel written in Bass

```python
import jax, jax.numpy as jnp
from jax import lax
import numpy as np

D_MODEL = 2048
BATCH = 1
SEQ = 16384
DEPTH = 1

EPS = 1e-6
N_MOD = 6
POOL_WINDOWS = (2, 4, 8, 16)
POOL_GROUPS = 4
POOL_WIDTH = D_MODEL
POOL_GDIM = POOL_WIDTH // POOL_GROUPS
RET_HEADS = 8
RET_DK = D_MODEL // RET_HEADS
RET_DV = 2 * RET_DK
RET_QK_WIDTH = RET_HEADS * RET_DK
RET_V_WIDTH = RET_HEADS * RET_DV
RET_CHUNK = 128
ROPE_BASE = 10000.0
SPLITS = (POOL_WIDTH, RET_QK_WIDTH, RET_QK_WIDTH, RET_V_WIDTH, RET_V_WIDTH, D_MODEL, D_MODEL)
IN_COLS = sum(SPLITS)
PEER_HEADS = 8
PEER_NKEYS = 128
PEER_N_EXPERTS = PEER_NKEYS * PEER_NKEYS
PEER_QDIM = 256
PEER_HALF = PEER_QDIM // 2
PEER_TOPK_HALF = 16
PEER_TOPK = 16
PEER_TOKEN_BLOCK = 128

kernel_name = 'hybrid_pool_retention_peer_adaln'


def rmsnorm(x, gain):
    xf = x.astype(jnp.float32)
    y = xf * lax.rsqrt(jnp.mean(xf * xf, axis=-1, keepdims=True) + EPS)
    return (y * gain.astype(jnp.float32)).astype(x.dtype)


def modulate(xn, shift, scale):
    return xn * (1 + scale[:, None, :]) + shift[:, None, :]


def head_layernorm(y, gain):
    B, S, H, dv = y.shape
    yf = y.astype(jnp.float32)
    mu = jnp.mean(yf, axis=-1, keepdims=True)
    var = jnp.mean(jnp.square(yf - mu), axis=-1, keepdims=True)
    yn = ((yf - mu) * lax.rsqrt(var + EPS)).reshape(B, S, H * dv)
    return (yn * gain.astype(jnp.float32)).astype(y.dtype)


def causal_window_mean(u, w):
    S = u.shape[1]
    cs = jnp.cumsum(u.astype(jnp.float32), axis=1)
    lag = jnp.pad(cs, ((0, 0), (w, 0), (0, 0)))[:, :S]
    count = jnp.minimum(jnp.arange(1, S + 1), w).astype(jnp.float32)
    return ((cs - lag) / count[None, :, None]).astype(u.dtype)


def pool_mixer(u, pool_w, pool_scale):
    B, S, _ = u.shape
    ug = u.reshape(B, S, POOL_GROUPS, POOL_GDIM)
    pooled = jnp.stack(
        [causal_window_mean(ug[:, :, g], w) - ug[:, :, g] for g, w in enumerate(POOL_WINDOWS)], axis=2)
    mixed = jnp.einsum('bsgc,gcd->bsgd', pooled, pool_w)
    return mixed.reshape(B, S, POOL_WIDTH) * pool_scale


def rotary(t, positions):
    dh = t.shape[-1]
    inv_freq = ROPE_BASE ** (-jnp.arange(0, dh, 2, dtype=jnp.float32) / dh)
    ang = positions.astype(jnp.float32)[..., None] * inv_freq
    cos = jnp.cos(ang)[:, :, None, :].astype(t.dtype)
    sin = jnp.sin(ang)[:, :, None, :].astype(t.dtype)
    t1, t2 = jnp.split(t, 2, axis=-1)
    return jnp.concatenate([t1 * cos - t2 * sin, t2 * cos + t1 * sin], axis=-1)


def retention(q, k, v):
    B, S, H, dk = q.shape
    dv = v.shape[-1]
    C = RET_CHUNK
    nC = S // C
    log_g = jnp.log(1.0 - 2.0 ** (-5.0 - jnp.arange(H, dtype=jnp.float32)))
    idx = jnp.arange(C, dtype=jnp.float32)
    diff = idx[:, None] - idx[None, :]
    decay_intra = jnp.where(diff[None] >= 0,
                            jnp.exp(jnp.maximum(diff, 0.0)[None] * log_g[:, None, None]), 0.0)
    q_dec = jnp.exp((idx + 1.0)[None, :] * log_g[:, None])
    k_dec = jnp.exp((C - 1.0 - idx)[None, :] * log_g[:, None])
    chunk_dec = jnp.exp(C * log_g)

    def to_chunks(t):
        return t.reshape(B, nC, C, H, t.shape[-1]).transpose(1, 0, 3, 2, 4)

    def step(state, inp):
        qi, ki, vi = inp
        scores = jnp.einsum('bhid,bhjd->bhij', qi, ki) * decay_intra
        inner = jnp.einsum('bhij,bhjv->bhiv', scores, vi)
        cross = jnp.einsum('bhid,bhdv->bhiv', qi * q_dec[None, :, :, None], state)
        new_state = state * chunk_dec[None, :, None, None] + jnp.einsum(
            'bhjd,bhjv->bhdv', ki * k_dec[None, :, :, None], vi)
        return new_state, inner + cross

    state0 = jnp.zeros((B, H, dk, dv), jnp.float32)
    _, out = lax.scan(step, state0, (to_chunks(q), to_chunks(k), to_chunks(v)))
    return out.transpose(1, 0, 3, 2, 4).reshape(B, S, H, dv).astype(v.dtype)


def peer_ffn(xn, w_query, sub_keys, expert_u, expert_v):
    B, S, D = xn.shape
    q = (xn @ w_query).reshape(B, S, PEER_HEADS, 2, PEER_HALF)
    s = jnp.einsum('bshpc,hpnc->bshpn', q, sub_keys)
    top_s, top_i = lax.top_k(s, PEER_TOPK_HALF)
    cand_s = (top_s[..., 0, :, None] + top_s[..., 1, None, :]).reshape(B, S, PEER_HEADS, -1)
    cand_e = (top_i[..., 0, :, None] * PEER_NKEYS + top_i[..., 1, None, :]).reshape(B, S, PEER_HEADS, -1)
    best_s, best_pos = lax.top_k(cand_s, PEER_TOPK)
    experts = jnp.take_along_axis(cand_e, best_pos, axis=-1)
    gates = jax.nn.softmax(best_s.astype(jnp.float32), axis=-1).astype(xn.dtype)
    T = B * S
    nb = T // PEER_TOKEN_BLOCK
    xt = xn.reshape(nb, PEER_TOKEN_BLOCK, D)
    et = experts.reshape(nb, PEER_TOKEN_BLOCK, PEER_HEADS * PEER_TOPK)
    gt = gates.reshape(nb, PEER_TOKEN_BLOCK, PEER_HEADS * PEER_TOPK)

    def block(args):
        xb, eb, gb = args
        u = expert_u[eb]
        v = expert_v[eb]
        act = jax.nn.gelu(jnp.einsum('td,tkd->tk', xb, u))
        return jnp.einsum('tk,tkd->td', gb * act, v)

    out = lax.map(block, (xt, et, gt))
    return out.reshape(B, S, D)


def setup_inputs(seed: int = 0) -> dict:
    key = jax.random.key(seed)
    ks = jax.random.split(key, 18)
    L = DEPTH
    D = D_MODEL

    def nrm(k, shape, scale):
        return jax.random.normal(k, shape, jnp.float32) * scale

    return {
        'x': nrm(ks[0], (BATCH, SEQ, D), 1.0),
        'c': nrm(ks[1], (BATCH, D), 1.0),
        'positions': jnp.broadcast_to(jnp.arange(SEQ, dtype=jnp.int32), (BATCH, SEQ)),
        'norm_mix_gain': 1.0 + nrm(ks[2], (L, D), 0.02),
        'w_ada': nrm(ks[3], (L, D, N_MOD * D), 0.5 * D ** -0.5),
        'b_ada': nrm(ks[4], (L, N_MOD * D), 0.02),
        'w_in': nrm(ks[5], (L, D, IN_COLS), D ** -0.5),
        'pool_w': nrm(ks[6], (L, POOL_GROUPS, POOL_GDIM, POOL_GDIM), POOL_GDIM ** -0.5),
        'pool_scale': 1.0 + nrm(ks[7], (L, POOL_WIDTH), 0.02),
        'ret_norm_gain': 1.0 + nrm(ks[8], (L, RET_V_WIDTH), 0.02),
        'w_branch_pool': nrm(ks[9], (L, POOL_WIDTH, D), POOL_WIDTH ** -0.5),
        'w_branch_ret': nrm(ks[10], (L, RET_V_WIDTH, D), RET_V_WIDTH ** -0.5),
        'w_out': nrm(ks[11], (L, D, D), D ** -0.5),
        'norm_ffn_gain': 1.0 + nrm(ks[12], (L, D), 0.02),
        'peer_w_query': nrm(ks[13], (L, D, PEER_HEADS * PEER_QDIM), D ** -0.5),
        'peer_sub_keys': nrm(ks[14], (L, PEER_HEADS, 2, PEER_NKEYS, PEER_HALF), PEER_HALF ** -0.5),
        'peer_u': nrm(ks[15], (L, PEER_N_EXPERTS, D), D ** -0.5),
        'peer_v': nrm(ks[16], (L, PEER_N_EXPERTS, D), PEER_HEADS ** -0.5),
        'final_norm_gain': 1.0 + nrm(ks[17], (D,), 0.02),
    }


def reference(x, c, positions, norm_mix_gain, w_ada, b_ada, w_in, pool_w, pool_scale, ret_norm_gain,
              w_branch_pool, w_branch_ret, w_out, norm_ffn_gain, peer_w_query, peer_sub_keys, peer_u, peer_v,
              final_norm_gain):
    B, S, _ = x.shape
    cond = jax.nn.silu(c)
    split_points = [int(p) for p in np.cumsum(SPLITS)[:-1]]
    for l in range(DEPTH):
        mod = cond @ w_ada[l] + b_ada[l]
        sh1, sc1, g1, sh2, sc2, g2 = jnp.split(mod, N_MOD, axis=-1)
        hn = modulate(rmsnorm(x, norm_mix_gain[l]), sh1, sc1)
        proj = hn @ w_in[l]
        u_pool, q, k, v, g_ret, a_pool, a_ret = jnp.split(proj, split_points, axis=-1)
        pool_out = pool_mixer(u_pool, pool_w[l], pool_scale[l])
        q = rotary(q.reshape(B, S, RET_HEADS, RET_DK), positions)
        k = rotary(k.reshape(B, S, RET_HEADS, RET_DK), positions) * (RET_DK ** -0.5)
        y = retention(q, k, v.reshape(B, S, RET_HEADS, RET_DV))
        ret_out = jax.nn.silu(g_ret) * head_layernorm(y, ret_norm_gain[l])
        merged = (jax.nn.sigmoid(a_pool) * (pool_out @ w_branch_pool[l])
                  + jax.nn.sigmoid(a_ret) * (ret_out @ w_branch_ret[l]))
        x = x + g1[:, None, :] * (merged @ w_out[l])
        fn = modulate(rmsnorm(x, norm_ffn_gain[l]), sh2, sc2)
        x = x + g2[:, None, :] * peer_ffn(fn, peer_w_query[l], peer_sub_keys[l], peer_u[l], peer_v[l])
    return rmsnorm(x, final_norm_gain)
```

```python
import numpy as np
import concourse.bass as bass
import concourse.mybir as mybir
from concourse.bass_utils import run_bass_kernel_spmd

F32 = mybir.dt.float32
BF16 = mybir.dt.bfloat16
I32 = mybir.dt.int32
U32 = mybir.dt.uint32
ALU = mybir.AluOpType
AF = mybir.ActivationFunctionType
AX = mybir.AxisListType

NCORES = 8
D = 2048
SEQ = 16384
TPC = SEQ // NCORES
NRING = 8


class Buf:
    def __init__(self, name, ap=None):
        self.name = name
        self.a = ap
        self.lw = None
        self.rd = {}

    def __getitem__(self, k):
        return self.a[k]


class Ctx:
    def __init__(self, nc, same_eng_sync=True):
        self.nc = nc
        self.eng = {'pe': nc.tensor, 'act': nc.scalar, 'dve': nc.vector, 'pool': nc.gpsimd, 'sp': nc.sync}
        self.cnt = {e: 0 for e in ('pe', 'act', 'dve', 'pool')}
        self.csem = {e: nc.alloc_semaphore('c_' + e) for e in ('pe', 'act', 'dve', 'pool')}
        self.ring = {q: [nc.alloc_semaphore('r_%s_%d' % (q, i)) for i in range(NRING)] for q in ('sp', 'act', 'pool')}
        self.dcnt = {q: 0 for q in ('sp', 'act', 'pool')}
        self.seen = {e: {} for e in self.eng}
        self.same = same_eng_sync
        self.nid = 0
        self.nwaits = 0
        self.ninstr = 0

    def sb(self, name, shape, dt=F32):
        self.nid += 1
        t = self.nc.alloc_sbuf_tensor('%s_%d' % (name, self.nid), list(shape), dt)
        return Buf(name, t.ap())

    def ps(self, name, shape, dt=F32):
        self.nid += 1
        t = self.nc.alloc_psum_tensor('%s_%d' % (name, self.nid), list(shape), dt)
        return Buf(name, t.ap())

    def dram(self, name, shape, dt=F32, kind="Internal"):
        t = self.nc.dram_tensor(name, list(shape), dt, kind=kind)
        return Buf(name, t.ap())

    def _sem(self, key):
        return self.csem[key[1]] if key[0] == 'c' else self.ring[key[1]][key[2]]

    def _wait(self, E, deps):
        seen = self.seen[E]
        for key, val in deps:
            if key[0] == 'c' and key[1] == E and (E == 'pe' or not self.same):
                continue
            if seen.get(key, 0) >= val:
                continue
            self.eng[E].wait_ge(self._sem(key), val)
            seen[key] = val
            self.nwaits += 1

    def _deps(self, reads, writes):
        deps = []
        for b in reads:
            if b.lw is not None:
                deps.append(b.lw)
        for b in writes:
            if b.lw is not None:
                deps.append(b.lw)
            deps.extend(b.rd.items())
        return deps

    def _commit(self, ev, reads, writes):
        key, val = ev
        for b in reads:
            if b.rd.get(key, 0) < val:
                b.rd[key] = val
        for b in writes:
            b.lw = ev
            b.rd = {}

    def op(self, E, fn, reads=(), writes=()):
        self._wait(E, self._deps(reads, writes))
        ins = fn(self.eng[E])
        self.cnt[E] += 1
        ins.then_inc(self.csem[E], 1)
        self.ninstr += 1
        self._commit((('c', E), self.cnt[E]), reads, writes)
        return ins

    def dma(self, q, fn, reads=(), writes=()):
        k = self.dcnt[q]
        s = k % NRING
        gen = k // NRING
        deps = self._deps(reads, writes)
        if gen > 0:
            deps.append((('d', q, s), 16 * gen))
        self._wait(q, deps)
        ins = fn(self.eng[q])
        ins.then_inc(self.ring[q][s], 16)
        self.dcnt[q] = k + 1
        self.ninstr += 1
        self._commit((('d', q, s), 16 * (gen + 1)), reads, writes)
        return ins

    def finish(self):
        for q in ('sp', 'act', 'pool'):
            k = self.dcnt[q]
            deps = []
            for s in range(NRING):
                n = (k - s + NRING - 1) // NRING
                if n > 0:
                    deps.append((('d', q, s), 16 * n))
            self._wait(q, deps)

    def barrier(self, skip_pool_dma=False):
        deps = [((('c', e)), self.cnt[e]) for e in self.cnt if self.cnt[e] > 0]
        for q in (('sp', 'act') if skip_pool_dma else ('sp', 'act', 'pool')):
            k = self.dcnt[q]
            for s in range(NRING):
                n = (k - s + NRING - 1) // NRING
                if n > 0:
                    deps.append((('d', q, s), 16 * n))
        for E in self.eng:
            self._wait(E, deps)


import math
PI = math.pi
HEADS = 8
DK = 256
DV = 512
GAM = [1.0 - 2.0 ** (-5.0 - h) for h in range(HEADS)]
POOLW = (2, 4, 8, 16)
HALO = 16
NGAT = 2
PREFIX_SKIP = 2.0 ** -40
EPS = 1e-6
NEXP = 16384
C_U, C_Q, C_K, C_V, C_G, C_AP, C_AR = 0, 2048, 4096, 6144, 10240, 14336, 16384
WIN_COLS = 18432


class Arena:
    def __init__(self, c, nbytes):
        self.c = c
        self.n = nbytes // 2
        c.nid += 1
        self.t = c.nc.alloc_sbuf_tensor('arena_%d' % c.nid, [128, self.n], BF16)
        self.off = 0

    def reset(self):
        self.off = 0

    def get(self, name, shape, dt=BF16):
        free = 1
        for s in shape[1:]:
            free *= s
        esz = 4 if dt in (F32, I32, U32) else 2
        nel2 = free * esz // 2
        self.off = (self.off + 1) // 2 * 2
        assert self.off + nel2 <= self.n, ("arena overflow", name, self.off, nel2, self.n)
        ap = self.t.ap()[0:shape[0], self.off:self.off + nel2]
        self.off += nel2
        if dt != BF16:
            ap = ap.bitcast(dt)
        if len(shape) == 3:
            ap = ap.rearrange("p (a b) -> p a b", a=shape[1])
        elif len(shape) == 4:
            ap = ap.rearrange("p (a b c) -> p a b c", a=shape[1], b=shape[2])
        return Buf(name, ap)


def const_layout(NT, NPRE):
    lay = {}
    o = 0
    for nm, w in (("ident", 128), ("dmaskT", 8 * 128), ("qdec", 8 * 128), ("kdown", 8), ("kdpre", 8 * NT),
                  ("coef", NPRE * 8), ("invf", 1), ("hm", 1), ("pcorr", 64), ("iota", 16)):
        lay[nm] = (o, w)
        o += w
    return lay, o


def build_nc(SEQ=16384, BLK=1024, stages="ABCD"):
    TPC = SEQ // NCORES
    NSUB = TPC // BLK
    NPRE = 7 * NSUB
    NT = BLK // 128
    MB = min(512, BLK)
    NMB = BLK // MB
    nc = bass.Bass("TRN2", target_bir_lowering=False)
    c = Ctx(nc)
    lay, NCONST = const_layout(NT, NPRE)

    def din(name, shape, dt=F32):
        return c.dram(name, shape, dt, kind="ExternalInput")

    x_own = din("x_own", [TPC, D]); x_pre = din("x_pre", [NPRE * BLK, D])
    pos_own = din("pos_own", [1, TPC], I32); pos_pre = din("pos_pre", [1, NPRE * BLK], I32)
    pvec1 = din("pvec1", [128, 128]); pvec2 = din("pvec2", [128, 128])
    b_ada = din("b_ada", [1, 6 * D]); fgain = din("fgain", [1, D])
    w_ada = din("w_ada", [D, 6 * D]); w_in = din("w_in", [D, WIN_COLS])
    pool_w = din("pool_w", [4 * 512, 512]); wbp = din("wbp", [D, D]); wbr = din("wbr", [2 * D, D]); wout = din("wout", [D, D])
    wq = din("wq", [D, D]); keys = din("keys", [16 * 128, 128])
    peer_u = din("peer_u", [NEXP, D]); peer_v = din("peer_v", [NEXP, D])
    consts_d = din("consts", [128, NCONST])
    out = c.dram("out", [TPC, D], F32, kind="ExternalOutput")
    win_b = c.dram("win_b", [D, WIN_COLS], BF16)
    poolw_b = c.dram("poolw_b", [4 * 512, 512], BF16)
    wbp_b = c.dram("wbp_b", [D, D], BF16); wbr_b = c.dram("wbr_b", [2 * D, D], BF16); wout_b = c.dram("wout_b", [D, D], BF16)
    ret_scr = c.dram("ret_scr", [32 * 128, TPC], BF16)
    x1_scr = c.dram("x1_scr", [TPC, D], F32)
    s_scr = c.dram("s_scr", [BLK, D], F32)
    g2row = c.dram("g2row", [1, D], F32)
    uv_bf = c.dram("uv_bf", [NEXP, 2 * D], BF16)
    fn_scr = c.dram("fn_scr", [BLK, D], BF16)

    cst = c.sb("consts", [128, NCONST])
    def CS(nm):
        o, w = lay[nm]
        return cst.a[:, o:o + w]
    ident = CS("ident")
    identb = c.sb("identb", [128, 128], BF16)
    vecT1 = c.sb("vecT1", [128, 128]); vecT2 = c.sb("vecT2", [128, 128])
    AB = c.sb("AB", [128, 4, 16])
    g1bc = c.sb("g1bc", [128, D])
    Sst = [c.sb("S%d" % h, [128, 2, 512]) for h in range(HEADS)]
    hn_raw = c.nc.alloc_sbuf_tensor("hn_raw", [128, 16 * (HALO + BLK)], BF16)
    hnT = Buf("hnT", hn_raw.ap().rearrange("p (k t) -> p k t", k=16))
    halo_save = c.sb("halo_save", [128, 16, HALO], BF16)
    small = c.sb("small", [128, 16])
    pb = [c.ps("pb%d" % i, [128, 512]) for i in range(8)]
    rr = {"proj": 0, "acc": 0, "tr": 0}
    def bank(role):
        base, n = {"proj": (0, 4), "acc": (4, 2), "tr": (6, 2)}[role]
        i = rr[role]; rr[role] = (i + 1) % n
        return pb[base + i]
    AR = Arena(c, 120 * 1024 + 512)

    ld = lambda dst, src, rd, q='sp': c.dma(q, lambda e: e.dma_start(out=dst.a if isinstance(dst, Buf) else dst[1], in_=src.a if isinstance(src, Buf) else src[1]),
                                            reads=[src if isinstance(src, Buf) else src[0]], writes=[dst if isinstance(dst, Buf) else dst[0]])

    ld(cst, consts_d, None)
    c.op('dve', lambda e: e.tensor_copy(out=identb.a, in_=ident), reads=[cst], writes=[identb])
    for h in range(HEADS):
        c.op('pool', lambda e, h=h: e.memset(Sst[h].a, 0.0), writes=[Sst[h]])
    c.op('pool', lambda e: e.memset(halo_save.a, 0.0), writes=[halo_save])
    def cast(dst, src, r0, r1, c0, c1):
        c.dma('pool', lambda e: e.dma_start(out=dst.a[r0:r1, c0:c1], in_=src.a[r0:r1, c0:c1]), reads=[src], writes=[dst])
    for r0 in range(0, D, 512):
        cast(win_b, w_in, r0, r0 + 512, C_K, C_G)

    late = []
    def _piece(dst, src, r0, r1, c0, c1, d0=None):
        d0 = c0 if d0 is None else d0
        late.append(lambda: c.dma('pool', lambda e: e.dma_start(out=dst.a[r0:r1, d0:d0 + (c1 - c0)], in_=src.a[r0:r1, c0:c1]), reads=[src], writes=[dst]))
    for (c0, c1) in ((C_Q, C_K), (C_G, C_AP), (C_U, C_Q), (C_AP, WIN_COLS)):
        for cc0 in range(c0, c1, 2048):
            for r0 in range(0, D, 512):
                _piece(win_b, w_in, r0, r0 + 512, cc0, cc0 + 2048)
    _piece(poolw_b, pool_w, 0, 2048, 0, 512)
    for r0 in range(0, D, 512):
        _piece(wbp_b, wbp, r0, r0 + 512, 0, D)
    for r0 in range(0, 2 * D, 512):
        _piece(wbr_b, wbr, r0, r0 + 512, 0, D)
    for r0 in range(0, D, 512):
        _piece(wout_b, wout, r0, r0 + 512, 0, D)
    n_weight_pieces = len(late)
    if "D" in stages:
        for r0 in range(0, NEXP, 512):
            _piece(uv_bf, peer_u, r0, r0 + 512, 0, D, d0=0)
            _piece(uv_bf, peer_v, r0, r0 + 512, 0, D, d0=D)
    late_i = [0]
    def trickle(n=1, upto=None):
        lim = len(late) if upto is None else min(upto, len(late))
        while n > 0 and late_i[0] < lim:
            late[late_i[0]]()
            late_i[0] += 1
            n -= 1

    AR.reset()
    pv = AR.get("pv", [128, 2, 128], F32)
    c.dma('sp', lambda e: e.dma_start(out=pv.a[:, 0, :], in_=pvec1.a), reads=[pvec1], writes=[pv])
    c.dma('sp', lambda e: e.dma_start(out=pv.a[:, 1, :], in_=pvec2.a), reads=[pvec2], writes=[pv])
    for j, vt in enumerate((vecT1, vecT2)):
        pt = bank("tr")
        c.op('pe', lambda e, j=j, pt=pt: e.transpose(out=pt.a[:, 0:128], in_=pv.a[:, j, :], identity=ident), reads=[pv, cst], writes=[pt])
        c.op('dve', lambda e, vt=vt, pt=pt: e.tensor_copy(out=vt.a, in_=pt.a[:, 0:128]), reads=[pt], writes=[vt])
    cond = AR.get("cond", [128, 16], F32)
    c.op('act', lambda e: e.activation(out=cond.a, in_=vecT2.a[:, 48:64], func=AF.Silu), reads=[vecT2], writes=[cond])
    condB = AR.get("condB", [128, 16, 128], F32)
    c.op('dve', lambda e: e.tensor_copy(out=condB.a, in_=cond.a.unsqueeze(2).to_broadcast([128, 16, 128])), reads=[cond], writes=[condB])
    g2bc = AR.get("g2bc", [128, D], F32)
    c.dma('sp', lambda e: e.dma_start(out=g1bc.a, in_=b_ada.a[0:1, 2 * D:3 * D].partition_broadcast(128)), reads=[b_ada], writes=[g1bc])
    c.dma('sp', lambda e: e.dma_start(out=g2bc.a, in_=b_ada.a[0:1, 5 * D:6 * D].partition_broadcast(128)), reads=[b_ada], writes=[g2bc])
    wab = [AR.get("wab%d" % i, [128, 16, 512], F32) for i in range(3)]
    modT = AR.get("modT", [128, 64], F32)
    pm = bank("acc")
    fm_blocks = {0: 0, 1: 1, 2: 2, 3: 3, 4: 4, 5: 5, 6: 6, 7: 7, 12: 8, 13: 9, 14: 10, 15: 11, 16: 12, 17: 13, 18: 14, 19: 15}
    for b in range(24):
        wb_ = wab[b % 3]
        c.dma('sp', lambda e, wb_=wb_, b=b: e.dma_start(out=wb_.a, in_=w_ada.a[:, b * 512:(b + 1) * 512].rearrange("(k p) n -> p k n", p=128)),
              reads=[w_ada], writes=[wb_])
        if b in fm_blocks:
            for jj in range(4):
                col = fm_blocks[b] * 4 + jj
                for kc in range(16):
                    c.op('pe', lambda e, wb_=wb_, jj=jj, kc=kc, col=col: e.matmul(pm.a[:, col:col + 1], lhsT=wb_.a[:, kc, jj * 128:(jj + 1) * 128],
                         rhs=cond.a[:, kc:kc + 1], start=(kc == 0), stop=(kc == 15)), reads=[wb_, cond], writes=[pm])
        else:
            tgt = g1bc if b < 12 else g2bc
            blk = (b - 8) if b < 12 else (b - 20)
            pg = bank("proj")
            for kc in range(16):
                c.op('pe', lambda e, wb_=wb_, kc=kc, pg=pg: e.matmul(pg.a[:, 0:512], lhsT=condB.a[:, kc, :], rhs=wb_.a[:, kc, :],
                     start=(kc == 0), stop=(kc == 15)), reads=[wb_, condB], writes=[pg])
            c.op('dve', lambda e, tgt=tgt, blk=blk, pg=pg: e.tensor_tensor(out=tgt.a[:, blk * 512:(blk + 1) * 512], in0=pg.a[:, 0:512],
                 in1=tgt.a[:, blk * 512:(blk + 1) * 512], op=ALU.add), reads=[pg, tgt], writes=[tgt])
    c.op('dve', lambda e: e.tensor_tensor(out=modT.a[:, 0:32], in0=pm.a[:, 0:32], in1=vecT1.a[:, 0:32], op=ALU.add), reads=[pm, vecT1], writes=[modT])
    c.op('dve', lambda e: e.tensor_tensor(out=modT.a[:, 32:64], in0=pm.a[:, 32:64], in1=vecT1.a[:, 48:80], op=ALU.add), reads=[pm, vecT1], writes=[modT])
    for i, (sc0, g0, sh0) in enumerate(((16, 96, 0), (48, 112, 32))):
        c.op('dve', lambda e, i=i, sc0=sc0, g0=g0: e.scalar_tensor_tensor(out=AB.a[:, 2 * i, :], in0=modT.a[:, sc0:sc0 + 16], scalar=1.0,
             in1=vecT1.a[:, g0:g0 + 16], op0=ALU.add, op1=ALU.mult), reads=[modT, vecT1], writes=[AB])
        c.op('dve', lambda e, i=i, sh0=sh0: e.tensor_copy(out=AB.a[:, 2 * i + 1, :], in_=modT.a[:, sh0:sh0 + 16]), reads=[modT], writes=[AB])
    c.dma('sp', lambda e: e.dma_start(out=g2row.a, in_=g2bc.a[0:1, :]), reads=[g2bc], writes=[g2row])
    c.barrier(skip_pool_dma=True)

    def stage_A(xsrc, r0):
        AR.reset()
        xtb = [AR.get("xt%d" % i, [128, D], F32) for i in range(2)]
        xsb = [AR.get("xs%d" % i, [128, D], BF16) for i in range(2)]
        junk = AR.get("junk", [128, D], BF16)
        stb = [AR.get("stA%d" % i, [128, 4], F32) for i in range(2)]
        c.op('dve', lambda e: e.tensor_copy(out=hnT.a[:, :, 0:HALO], in_=halo_save.a), reads=[halo_save], writes=[hnT])
        for t in range(NT):
            xt = xtb[t % 2]; xs = xsb[t % 2]; st = stb[t % 2]
            c.dma('sp', lambda e, xt=xt, t=t: e.dma_start(out=xt.a, in_=xsrc.a[r0 + t * 128:r0 + (t + 1) * 128, :]), reads=[xsrc], writes=[xt])
            c.op('dve', lambda e: e.memset(st.a[:, 0:1], 0.0), writes=[st])
            c.op('act', lambda e, xt=xt: e.activation(out=junk.a, in_=xt.a, func=AF.Square, accum_out=st.a[:, 0:1]), reads=[xt, st], writes=[junk, st])
            c.op('dve', lambda e: e.tensor_scalar(out=st.a[:, 1:2], in0=st.a[:, 0:1], scalar1=1.0 / D, scalar2=EPS, op0=ALU.mult, op1=ALU.add), reads=[st], writes=[st])
            c.op('act', lambda e: e.activation(out=st.a[:, 2:3], in_=st.a[:, 1:2], func=AF.Sqrt), reads=[st], writes=[st])
            c.op('dve', lambda e: e.reciprocal(out=st.a[:, 3:4], in_=st.a[:, 2:3]), reads=[st], writes=[st])
            c.op('dve', lambda e, xt=xt: e.tensor_scalar(out=xs.a, in0=xt.a, scalar1=st.a[:, 3:4], scalar2=None, op0=ALU.mult), reads=[xt, st], writes=[xs])
            for half in range(2):
                pt = bank("tr"); ptb = pt.a.bitcast(BF16)
                for j in range(8):
                    kc = half * 8 + j
                    c.op('pe', lambda e, kc=kc, j=j, ptb=ptb: e.transpose(out=ptb[:, j * 128:(j + 1) * 128], in_=xs.a[:, kc * 128:(kc + 1) * 128], identity=identb.a),
                         reads=[xs, identb], writes=[pt])
                for j in range(8):
                    kc = half * 8 + j
                    if j % 2 == 0:
                        c.op('act', lambda e, kc=kc, j=j, ptb=ptb, t=t: e.activation(out=hnT.a[:, kc, HALO + t * 128:HALO + (t + 1) * 128], in_=ptb[:, j * 128:(j + 1) * 128],
                             func=AF.Identity, scale=AB.a[:, 0, kc:kc + 1], bias=AB.a[:, 1, kc:kc + 1]), reads=[pt, AB], writes=[hnT])
                    else:
                        c.op('dve', lambda e, kc=kc, j=j, ptb=ptb, t=t: e.tensor_scalar(out=hnT.a[:, kc, HALO + t * 128:HALO + (t + 1) * 128], in0=ptb[:, j * 128:(j + 1) * 128],
                             scalar1=AB.a[:, 0, kc:kc + 1], scalar2=AB.a[:, 1, kc:kc + 1], op0=ALU.mult, op1=ALU.add), reads=[pt, AB], writes=[hnT])

        c.op('dve', lambda e: e.tensor_copy(out=halo_save.a, in_=hnT.a[:, :, BLK:BLK + HALO]), reads=[hnT], writes=[halo_save])

    def rope_tables(psrc, p0, cs):
        C1 = 6.28125; C2 = 2 * PI - 6.28125
        pi_ = AR.get("pi", [128, BLK], I32); ang = AR.get("ang", [128, BLK], F32)
        kf = AR.get("kf", [128, BLK], F32); m = AR.get("m", [128, BLK], F32)
        invf = CS("invf")
        c.dma('sp', lambda e: e.dma_start(out=pi_.a, in_=psrc.a[0:1, p0:p0 + BLK].partition_broadcast(128)), reads=[psrc], writes=[pi_])
        c.op('dve', lambda e: e.tensor_copy(out=kf.a, in_=pi_.a), reads=[pi_], writes=[kf])
        c.op('dve', lambda e: e.tensor_scalar(out=ang.a, in0=kf.a, scalar1=invf, scalar2=None, op0=ALU.mult), reads=[kf, cst], writes=[ang])
        for j, sh in enumerate([0.0, 0.5 * PI]):
            r_ = cs.a[:, j, :]
            c.op('dve', lambda e: e.tensor_scalar(out=kf.a, in0=ang.a, scalar1=sh, scalar2=1.0 / (2 * PI), op0=ALU.add, op1=ALU.mult), reads=[ang], writes=[kf])
            c.op('dve', lambda e: e.tensor_copy(out=pi_.a, in_=kf.a), reads=[kf], writes=[pi_])
            c.op('dve', lambda e: e.tensor_copy(out=kf.a, in_=pi_.a), reads=[pi_], writes=[kf])
            c.op('dve', lambda e, r_=r_: e.scalar_tensor_tensor(out=r_, in0=kf.a, scalar=-C1, in1=ang.a, op0=ALU.mult, op1=ALU.add), reads=[kf, ang], writes=[cs])
            c.op('dve', lambda e, r_=r_: e.scalar_tensor_tensor(out=r_, in0=kf.a, scalar=-C2, in1=r_, op0=ALU.mult, op1=ALU.add), reads=[kf, cs], writes=[cs])
            if sh != 0.0:
                c.op('dve', lambda e, r_=r_: e.tensor_scalar(out=r_, in0=r_, scalar1=sh, scalar2=None, op0=ALU.add), reads=[cs], writes=[cs])
            c.op('dve', lambda e, r_=r_: e.tensor_single_scalar(out=m.a, in_=r_, scalar=PI, op=ALU.is_gt), reads=[cs], writes=[m])
            c.op('dve', lambda e, r_=r_: e.scalar_tensor_tensor(out=r_, in0=m.a, scalar=-2 * PI, in1=r_, op0=ALU.mult, op1=ALU.add), reads=[m, cs], writes=[cs])
            c.op('dve', lambda e, r_=r_: e.tensor_single_scalar(out=m.a, in_=r_, scalar=-PI, op=ALU.is_lt), reads=[cs], writes=[m])
            c.op('dve', lambda e, r_=r_: e.scalar_tensor_tensor(out=r_, in0=m.a, scalar=2 * PI, in1=r_, op0=ALU.mult, op1=ALU.add), reads=[m, cs], writes=[cs])
        c.op('act', lambda e: e.activation(out=cs.a, in_=cs.a, func=AF.Sin), reads=[cs], writes=[cs])

    def load_w(dst_ap, dstbuf, src, c0, ncols, r0=0, nk=16):
        c.dma('sp', lambda e: e.dma_start(out=dst_ap, in_=src.a[r0:r0 + nk * 128, c0:c0 + ncols].rearrange("(k p) n -> p k n", p=128)),
              reads=[src], writes=[dstbuf])

    def proj_rot(wbuf, wc0, cs, dstT, rt):
        for mb in range(NMB):
            P = []
            for cc in range(2):
                ps = bank("proj")
                for kc in range(16):
                    c.op('pe', lambda e, ps=ps, kc=kc, cc=cc, mb=mb: e.matmul(ps.a[:, 0:MB], lhsT=wbuf.a[:, kc, wc0 + cc * 128:wc0 + (cc + 1) * 128],
                         rhs=hnT.a[:, kc, HALO + mb * MB:HALO + (mb + 1) * MB], start=(kc == 0), stop=(kc == 15)), reads=[wbuf, hnT], writes=[ps])
                P.append(ps)
            sl = slice(mb * MB, (mb + 1) * MB)
            sin_, cos_ = cs.a[:, 0, sl], cs.a[:, 1, sl]
            tt = lambda o, a, b, op, rd, wr: c.op('dve', lambda e: e.tensor_tensor(out=o, in0=a, in1=b, op=op), reads=rd, writes=wr)
            tt(rt[0].a, P[0].a[:, 0:MB], cos_, ALU.mult, [P[0], cs], [rt[0]])
            tt(rt[1].a, P[1].a[:, 0:MB], sin_, ALU.mult, [P[1], cs], [rt[1]])
            tt(dstT.a[:, 0, sl], rt[0].a, rt[1].a, ALU.subtract, [rt[0], rt[1]], [dstT])
            tt(rt[2].a, P[1].a[:, 0:MB], cos_, ALU.mult, [P[1], cs], [rt[2]])
            tt(rt[3].a, P[0].a[:, 0:MB], sin_, ALU.mult, [P[0], cs], [rt[3]])
            tt(dstT.a[:, 1, sl], rt[2].a, rt[3].a, ALU.add, [rt[2], rt[3]], [dstT])

    def k_tok(kT, k_s, kd_ap_fn):
        for t in range(NT):
            pt = bank("tr"); ptb = pt.a.bitcast(BF16)
            for cc in range(2):
                c.op('pe', lambda e, cc=cc, t=t, ptb=ptb: e.transpose(out=ptb[:, cc * 128:(cc + 1) * 128], in_=kT.a[:, cc, t * 128:(t + 1) * 128], identity=identb.a),
                     reads=[kT, identb], writes=[pt])
            c.op('act', lambda e, t=t, ptb=ptb: e.activation(out=k_s.a[:, t, :], in_=ptb[:, 0:256], func=AF.Copy, scale=kd_ap_fn(t)), reads=[pt, cst], writes=[k_s])

    def v_proj(wbuf, v_tok):
        for t in range(NT):
            ps = bank("proj")
            for kc in range(16):
                c.op('pe', lambda e, ps=ps, kc=kc, t=t: e.matmul(ps.a[:, 0:512], lhsT=hnT.a[:, kc, HALO + t * 128:HALO + (t + 1) * 128], rhs=wbuf.a[:, kc, :],
                     start=(kc == 0), stop=(kc == 15)), reads=[wbuf, hnT], writes=[ps])
            c.op('act', lambda e, ps=ps, t=t: e.activation(out=v_tok.a[:, t, :], in_=ps.a[:, 0:512], func=AF.Copy), reads=[ps], writes=[v_tok])

    def alloc_B():
        AR.reset()
        B = {}
        B["w0"] = AR.get("w0", [128, 16, 512]); B["w1"] = AR.get("w1", [128, 16, 512]); B["w2"] = AR.get("w2", [128, 16, 512])
        B["kT"] = AR.get("kT", [128, 2, BLK]); B["qT"] = AR.get("qT", [128, 2, BLK]); B["qTs"] = AR.get("qTs", [128, 2, BLK])
        B["k_s"] = AR.get("k_s", [128, NT, 256]); B["v_tok"] = AR.get("v_tok", [128, NT, 512]); B["gTs"] = AR.get("gTs", [128, 4, BLK])
        B["cs"] = AR.get("cs", [128, 2, BLK], F32)
        B["rt"] = [AR.get("rt%d" % i, [128, MB], F32) for i in range(4)]
        B["Sbf"] = AR.get("Sbf", [128, 2, 512])
        B["yn"] = AR.get("yn", [128, 512]); B["sT"] = AR.get("sT", [128, 128]); B["rT"] = AR.get("rT", [128, 4, 128])
        B["ln"] = AR.get("ln", [128, 12], F32)
        return B

    def alloc_P():
        AR.reset()
        P = {}
        P["wk"] = [AR.get("wk%d" % i, [128, 16, 256]) for i in range(2)]
        P["wv"] = [AR.get("wv%d" % i, [128, 16, 512]) for i in range(2)]
        P["kT"] = AR.get("kT", [128, 2, BLK]); P["k_s"] = AR.get("k_s", [128, NT, 256]); P["v_tok"] = AR.get("v_tok", [128, NT, 512])
        P["cs"] = AR.get("cs", [128, 2, BLK], F32)
        P["rt"] = [AR.get("rt%d" % i, [128, MB], F32) for i in range(4)]
        return P

    if "A" in stages:
        for p in range(NPRE):
            stage_A(x_pre, p * BLK)
            c.barrier(skip_pool_dma=True)
            P = alloc_P()
            def ldkv(h):
                load_w(P["wk"][h % 2].a, P["wk"][h % 2], win_b, C_K + h * 256, 256)
                load_w(P["wv"][h % 2].a, P["wv"][h % 2], win_b, C_V + h * 512, 512)
            ldkv(0)
            rope_tables(pos_pre, p * BLK, P["cs"])
            for h in range(HEADS):
                if h + 1 < HEADS:
                    ldkv(h + 1)
                if p >= 1:
                    trickle(2 if p >= NPRE - 2 else 1)
                if GAM[h] ** (BLK * (NPRE - 1 - p)) < PREFIX_SKIP:
                    continue
                wk_, wv_ = P["wk"][h % 2], P["wv"][h % 2]
                proj_rot(wk_, 0, P["cs"], P["kT"], P["rt"])
                v_proj(wv_, P["v_tok"])
                kd0 = lay["kdpre"][0]
                k_tok(P["kT"], P["k_s"], lambda t, h=h: cst.a[:, kd0 + h * NT + t:kd0 + h * NT + t + 1])
                co0 = lay["coef"][0]
                for cc in range(2):
                    pa = bank("acc")
                    for t in range(NT):
                        c.op('pe', lambda e, pa=pa, t=t, cc=cc: e.matmul(pa.a[:, 0:512], lhsT=P["k_s"].a[:, t, cc * 128:(cc + 1) * 128], rhs=P["v_tok"].a[:, t, :],
                             start=(t == 0), stop=(t == NT - 1)), reads=[P["k_s"], P["v_tok"]], writes=[pa])
                    c.op('dve', lambda e, pa=pa, cc=cc, h=h, p=p: e.scalar_tensor_tensor(out=Sst[h].a[:, cc, :], in0=pa.a[:, 0:512],
                         scalar=cst.a[:, co0 + p * 8 + h:co0 + p * 8 + h + 1], in1=Sst[h].a[:, cc, :], op0=ALU.mult, op1=ALU.add), reads=[pa, cst, Sst[h]], writes=[Sst[h]])
            c.barrier(skip_pool_dma=True)

    trickle(10 ** 9, upto=n_weight_pieces)
    for s in range(NSUB):
        tok0 = s * BLK
        stage_A(x_own, tok0)
        c.barrier(skip_pool_dma=(s == 0))
        B = alloc_B()
        rope_tables(pos_own, tok0, B["cs"])
        dm0 = lay["dmaskT"][0]; qd0 = lay["qdec"][0]; ko0 = lay["kdown"][0]
        def ld_qk(h):
            load_w(B["w0"].a[:, :, 0:256], B["w0"], win_b, C_Q + h * 256, 256)
            load_w(B["w0"].a[:, :, 256:512], B["w0"], win_b, C_K + h * 256, 256)
        def ld_v(h):
            load_w(B["w1"].a, B["w1"], win_b, C_V + h * 512, 512)
        def ld_g(h):
            load_w(B["w2"].a, B["w2"], win_b, C_G + h * 512, 512)
        for h in range(HEADS):
            if h == 0:
                ld_qk(0); ld_v(0); ld_g(0)
            if s == 0:
                trickle(4)
            proj_rot(B["w0"], 0, B["cs"], B["qT"], B["rt"])
            proj_rot(B["w0"], 256, B["cs"], B["kT"], B["rt"])
            if h + 1 < HEADS:
                ld_qk(h + 1)
            v_proj(B["w1"], B["v_tok"])
            if h + 1 < HEADS:
                ld_v(h + 1)
            for cc in range(2):
                c.op('dve', lambda e, cc=cc, h=h: e.tensor_tensor(out=B["qTs"].a[:, cc, :].rearrange("p (n i) -> p n i", i=128),
                     in0=B["qT"].a[:, cc, :].rearrange("p (n i) -> p n i", i=128),
                     in1=cst.a[:, qd0 + h * 128:qd0 + (h + 1) * 128].unsqueeze(1).to_broadcast([128, NT, 128]), op=ALU.mult),
                     reads=[B["qT"], cst], writes=[B["qTs"]])
            k_tok(B["kT"], B["k_s"], lambda t, h=h: cst.a[:, ko0 + h:ko0 + h + 1])
            for vc in range(4):
                for mb in range(NMB):
                    ps = bank("proj")
                    for kc in range(16):
                        c.op('pe', lambda e, ps=ps, kc=kc, vc=vc, mb=mb: e.matmul(ps.a[:, 0:MB], lhsT=B["w2"].a[:, kc, vc * 128:(vc + 1) * 128],
                             rhs=hnT.a[:, kc, HALO + mb * MB:HALO + (mb + 1) * MB], start=(kc == 0), stop=(kc == 15)), reads=[B["w2"], hnT], writes=[ps])
                    c.op('act', lambda e, ps=ps, vc=vc, mb=mb: e.activation(out=B["gTs"].a[:, vc, mb * MB:(mb + 1) * MB], in_=ps.a[:, 0:MB], func=AF.Silu),
                         reads=[ps], writes=[B["gTs"]])
            if h + 1 < HEADS:
                ld_g(h + 1)
            c.op('act', lambda e, h=h: e.activation(out=B["Sbf"].a, in_=Sst[h].a, func=AF.Copy), reads=[Sst[h]], writes=[B["Sbf"]])
            for n in range(NT):
                sl = slice(n * 128, (n + 1) * 128)
                psc = bank("tr")
                for cc in range(2):
                    c.op('pe', lambda e, cc=cc, sl=sl, psc=psc: e.matmul(psc.a[:, 0:128], lhsT=B["kT"].a[:, cc, sl], rhs=B["qT"].a[:, cc, sl],
                         start=(cc == 0), stop=(cc == 1)), reads=[B["kT"], B["qT"]], writes=[psc])
                c.op('dve', lambda e, psc=psc, h=h: e.tensor_tensor(out=B["sT"].a, in0=psc.a[:, 0:128], in1=cst.a[:, dm0 + h * 128:dm0 + (h + 1) * 128], op=ALU.mult),
                     reads=[psc, cst], writes=[B["sT"]])
                po = bank("proj")
                c.op('pe', lambda e, po=po, n=n: e.matmul(po.a[:, 0:512], lhsT=B["sT"].a, rhs=B["v_tok"].a[:, n, :], start=True, stop=False),
                     reads=[B["sT"], B["v_tok"]], writes=[po])
                for cc in range(2):
                    c.op('pe', lambda e, po=po, cc=cc, sl=sl: e.matmul(po.a[:, 0:512], lhsT=B["qTs"].a[:, cc, sl], rhs=B["Sbf"].a[:, cc, :], start=False, stop=(cc == 1)),
                         reads=[B["qTs"], B["Sbf"]], writes=[po])
                for cc in range(2):
                    pa = bank("acc")
                    c.op('pe', lambda e, pa=pa, cc=cc, n=n: e.matmul(pa.a[:, 0:512], lhsT=B["k_s"].a[:, n, cc * 128:(cc + 1) * 128], rhs=B["v_tok"].a[:, n, :], start=True, stop=True),
                         reads=[B["k_s"], B["v_tok"]], writes=[pa])
                    c.op('dve', lambda e, pa=pa, cc=cc, h=h: e.scalar_tensor_tensor(out=Sst[h].a[:, cc, :], in0=Sst[h].a[:, cc, :], scalar=float(GAM[h] ** 128),
                         in1=pa.a[:, 0:512], op0=ALU.mult, op1=ALU.add), reads=[pa, Sst[h]], writes=[Sst[h]])
                c.op('act', lambda e, h=h: e.activation(out=B["Sbf"].a, in_=Sst[h].a, func=AF.Copy), reads=[Sst[h]], writes=[B["Sbf"]])
                ln = B["ln"]
                c.op('dve', lambda e, po=po: e.bn_stats(out=ln.a[:, 0:6], in_=po.a[:, 0:512]), reads=[po], writes=[ln])
                c.op('dve', lambda e: e.bn_aggr(out=ln.a[:, 6:8], in_=ln.a[:, 0:6]), reads=[ln], writes=[ln])
                c.op('dve', lambda e: e.tensor_scalar(out=ln.a[:, 8:9], in0=ln.a[:, 7:8], scalar1=EPS, scalar2=None, op0=ALU.add), reads=[ln], writes=[ln])
                c.op('act', lambda e: e.activation(out=ln.a[:, 9:10], in_=ln.a[:, 8:9], func=AF.Sqrt), reads=[ln], writes=[ln])
                c.op('dve', lambda e: e.reciprocal(out=ln.a[:, 10:11], in_=ln.a[:, 9:10]), reads=[ln], writes=[ln])
                c.op('dve', lambda e, po=po: e.tensor_scalar(out=B["yn"].a, in0=po.a[:, 0:512], scalar1=ln.a[:, 6:7], scalar2=ln.a[:, 10:11], op0=ALU.subtract, op1=ALU.mult),
                     reads=[po, ln], writes=[B["yn"]])
                pt = bank("tr"); ptb = pt.a.bitcast(BF16)
                for vc in range(4):
                    c.op('pe', lambda e, vc=vc, ptb=ptb: e.transpose(out=ptb[:, vc * 128:(vc + 1) * 128], in_=B["yn"].a[:, vc * 128:(vc + 1) * 128], identity=identb.a),
                         reads=[B["yn"], identb], writes=[pt])
                for vc in range(4):
                    c.op('dve', lambda e, vc=vc, ptb=ptb, h=h, sl=sl: e.scalar_tensor_tensor(out=B["rT"].a[:, vc, :], in0=ptb[:, vc * 128:(vc + 1) * 128],
                         scalar=vecT2.a[:, 16 + h * 4 + vc:17 + h * 4 + vc], in1=B["gTs"].a[:, vc, sl], op0=ALU.mult, op1=ALU.mult), reads=[pt, vecT2, B["gTs"]], writes=[B["rT"]])
                c.dma('sp', lambda e, h=h, n=n: e.dma_start(out=ret_scr.a[h * 512:(h + 1) * 512, tok0 + n * 128:tok0 + (n + 1) * 128].rearrange("(v p) t -> p v t", p=128),
                      in_=B["rT"].a), reads=[B["rT"]], writes=[ret_scr])
        c.barrier(skip_pool_dma=(s == 0))
        if "C" not in stages:
            continue
        for tb in range(NMB):
            AR.reset()
            W = [AR.get("cw%d" % i, [128, 16, 512]) for i in range(3)]
            seq = []
            for g_ in range(4):
                seq.append((win_b, C_U + g_ * 512, 0))
            for nb_ in range(4):
                seq += [(wbp_b, nb_ * 512, 0), (win_b, C_AP + nb_ * 512, 0), (wbr_b, nb_ * 512, 0), (wbr_b, nb_ * 512, 2048), (win_b, C_AR + nb_ * 512, 0)]
            for db_ in range(4):
                seq.append((wout_b, db_ * 512, 0))
            wst = {"ptr": 0, "issued": 0}
            def nextw():
                i = wst["ptr"]
                while wst["issued"] <= min(i + 1, len(seq) - 1):
                    j = wst["issued"]
                    src_, c0_, r0_ = seq[j]
                    load_w(W[j % 3].a, W[j % 3], src_, c0_, 512, r0=r0_)
                    wst["issued"] += 1
                wst["ptr"] += 1
                return W[i % 3]
            U = AR.get("U", [128, HALO + MB], F32); ta = AR.get("ta", [128, HALO + MB], F32); tb_ = AR.get("tb", [128, HALO + MB], F32)
            pooledT = AR.get("pooledT", [128, 4, MB]); pw = AR.get("pw", [128, 4, 512])
            pool_outT = AR.get("pool_outT", [128, 16, MB]); mergedT = AR.get("mergedT", [128, 16, MB])
            retT = AR.get("retT", [128, 16, MB])
            ga = AR.get("ga", [128, MB]); gr = AR.get("gr", [128, MB]); t1 = AR.get("t1", [128, MB], F32); t2 = AR.get("t2", [128, MB], F32)
            xp = [AR.get("xp%d" % i, [128, 512], F32) for i in range(2)]
            hc0 = tb * MB
            first = (s == 0 and tb == 0)
            pc0 = lay["pcorr"][0]; hm_ap = CS("hm")
            for g in range(4):
                w_ = POOLW[g]
                wu = nextw()
                load_w(pw.a, pw, poolw_b, 0, 512, r0=g * 512, nk=4)
                for cc in range(4):
                    ph = bank("acc"); pm_ = bank("proj")
                    for kc in range(16):
                        c.op('pe', lambda e, kc=kc, cc=cc, ph=ph: e.matmul(ph.a[:, 0:HALO], lhsT=wu.a[:, kc, cc * 128:(cc + 1) * 128], rhs=hnT.a[:, kc, hc0:hc0 + HALO],
                             start=(kc == 0), stop=(kc == 15)), reads=[wu, hnT], writes=[ph])
                    for kc in range(16):
                        c.op('pe', lambda e, kc=kc, cc=cc, pm_=pm_: e.matmul(pm_.a[:, 0:MB], lhsT=wu.a[:, kc, cc * 128:(cc + 1) * 128], rhs=hnT.a[:, kc, hc0 + HALO:hc0 + HALO + MB],
                             start=(kc == 0), stop=(kc == 15)), reads=[wu, hnT], writes=[pm_])
                    if first:
                        c.op('dve', lambda e, ph=ph: e.tensor_scalar(out=U.a[:, 0:HALO], in0=ph.a[:, 0:HALO], scalar1=hm_ap, scalar2=None, op0=ALU.mult), reads=[ph, cst], writes=[U])
                    else:
                        c.op('act', lambda e, ph=ph: e.activation(out=U.a[:, 0:HALO], in_=ph.a[:, 0:HALO], func=AF.Copy), reads=[ph], writes=[U])
                    c.op('act', lambda e, pm_=pm_: e.activation(out=U.a[:, HALO:], in_=pm_.a[:, 0:MB], func=AF.Copy), reads=[pm_], writes=[U])
                    L = HALO + MB
                    src, dst, sh = U, ta, 1
                    while sh < w_:
                        c.op('dve', lambda e, src=src, dst=dst, sh=sh: e.tensor_tensor(out=dst.a[:, 2 * sh - 1:L], in0=src.a[:, 2 * sh - 1:L], in1=src.a[:, sh - 1:L - sh], op=ALU.add),
                             reads=[src], writes=[dst])
                        src = dst
                        dst = tb_ if dst is ta else ta
                        sh *= 2
                    if first:
                        c.op('dve', lambda e, src=src, g=g: e.tensor_tensor(out=src.a[:, HALO:2 * HALO], in0=src.a[:, HALO:2 * HALO], in1=cst.a[:, pc0 + g * 16:pc0 + (g + 1) * 16], op=ALU.mult),
                             reads=[src, cst], writes=[src])
                    c.op('dve', lambda e, src=src, cc=cc: e.scalar_tensor_tensor(out=pooledT.a[:, cc, :], in0=src.a[:, HALO:], scalar=1.0 / w_, in1=U.a[:, HALO:], op0=ALU.mult, op1=ALU.subtract),
                         reads=[src, U], writes=[pooledT])
                for dc in range(4):
                    pq = bank("proj")
                    for k4 in range(4):
                        c.op('pe', lambda e, pq=pq, k4=k4, dc=dc: e.matmul(pq.a[:, 0:MB], lhsT=pw.a[:, k4, dc * 128:(dc + 1) * 128], rhs=pooledT.a[:, k4, :], start=(k4 == 0), stop=(k4 == 3)),
                             reads=[pw, pooledT], writes=[pq])
                    c.op('act', lambda e, pq=pq, dc=dc, g=g: e.activation(out=pool_outT.a[:, g * 4 + dc, :], in_=pq.a[:, 0:MB], func=AF.Copy, scale=vecT2.a[:, g * 4 + dc:g * 4 + dc + 1]),
                         reads=[pq, vecT2], writes=[pool_outT])
            for nb in range(4):
                wp = nextw()
                wa = nextw()
                T1 = []
                for nn in range(4):
                    pp = bank("proj")
                    for kc in range(16):
                        c.op('pe', lambda e, pp=pp, kc=kc, nn=nn: e.matmul(pp.a[:, 0:MB], lhsT=wp.a[:, kc, nn * 128:(nn + 1) * 128], rhs=pool_outT.a[:, kc, :], start=(kc == 0), stop=(kc == 15)),
                             reads=[wp, pool_outT], writes=[pp])
                    pa = bank("acc")
                    for kc in range(16):
                        c.op('pe', lambda e, pa=pa, kc=kc, nn=nn: e.matmul(pa.a[:, 0:MB], lhsT=wa.a[:, kc, nn * 128:(nn + 1) * 128], rhs=hnT.a[:, kc, hc0 + HALO:hc0 + HALO + MB], start=(kc == 0), stop=(kc == 15)),
                             reads=[wa, hnT], writes=[pa])
                    c.op('act', lambda e, pa=pa: e.activation(out=ga.a, in_=pa.a[:, 0:MB], func=AF.Sigmoid), reads=[pa], writes=[ga])
                    c.op('dve', lambda e, pp=pp, nn=nn, nb=nb: e.tensor_tensor(out=mergedT.a[:, nb * 4 + nn, :], in0=pp.a[:, 0:MB], in1=ga.a, op=ALU.mult), reads=[pp, ga], writes=[mergedT])
                PR = [bank("proj") for _ in range(4)]
                for kh in range(2):
                    wr_ = nextw()
                    c.dma('sp', lambda e, kh=kh: e.dma_start(out=retT.a, in_=ret_scr.a[kh * 2048:(kh + 1) * 2048, tok0 + tb * MB:tok0 + (tb + 1) * MB].rearrange("(k p) t -> p k t", p=128)),
                          reads=[ret_scr], writes=[retT])
                    for nn in range(4):
                        for kc in range(16):
                            c.op('pe', lambda e, nn=nn, kc=kc, kh=kh, wr_=wr_: e.matmul(PR[nn].a[:, 0:MB], lhsT=wr_.a[:, kc, nn * 128:(nn + 1) * 128], rhs=retT.a[:, kc, :],
                                 start=(kh == 0 and kc == 0), stop=(kh == 1 and kc == 15)), reads=[wr_, retT], writes=[PR[nn]])
                wa2 = nextw()
                for nn in range(4):
                    pa = bank("acc")
                    for kc in range(16):
                        c.op('pe', lambda e, pa=pa, kc=kc, nn=nn: e.matmul(pa.a[:, 0:MB], lhsT=wa2.a[:, kc, nn * 128:(nn + 1) * 128], rhs=hnT.a[:, kc, hc0 + HALO:hc0 + HALO + MB], start=(kc == 0), stop=(kc == 15)),
                             reads=[wa2, hnT], writes=[pa])
                    c.op('act', lambda e, pa=pa: e.activation(out=gr.a, in_=pa.a[:, 0:MB], func=AF.Sigmoid), reads=[pa], writes=[gr])
                    c.op('dve', lambda e, nn=nn: e.tensor_tensor(out=t2.a, in0=PR[nn].a[:, 0:MB], in1=gr.a, op=ALU.mult), reads=[PR[nn], gr], writes=[t2])
                    c.op('dve', lambda e, nn=nn, nb=nb: e.tensor_tensor(out=mergedT.a[:, nb * 4 + nn, :], in0=mergedT.a[:, nb * 4 + nn, :], in1=t2.a, op=ALU.add), reads=[mergedT, t2], writes=[mergedT])
            for db in range(4):
                wo = nextw()
                for tt in range(MB // 128):
                    r0 = tok0 + tb * MB + tt * 128
                    py = bank("proj")
                    for kc in range(16):
                        c.op('pe', lambda e, py=py, kc=kc, tt=tt: e.matmul(py.a[:, 0:512], lhsT=mergedT.a[:, kc, tt * 128:(tt + 1) * 128], rhs=wo.a[:, kc, :], start=(kc == 0), stop=(kc == 15)),
                             reads=[mergedT, wo], writes=[py])
                    xq = xp[(db * 4 + tt) % 2]
                    c.dma('sp', lambda e, xq=xq, r0=r0, db=db: e.dma_start(out=xq.a, in_=x_own.a[r0:r0 + 128, db * 512:(db + 1) * 512]), reads=[x_own], writes=[xq])
                    c.op('dve', lambda e, py=py, db=db: e.tensor_tensor(out=t1.a[:, 0:512] if MB >= 512 else t1.a, in0=py.a[:, 0:512], in1=g1bc.a[:, db * 512:(db + 1) * 512], op=ALU.mult), reads=[py, g1bc], writes=[t1])
                    c.op('dve', lambda e, xq=xq: e.tensor_tensor(out=xq.a, in0=xq.a, in1=t1.a[:, 0:512], op=ALU.add), reads=[xq, t1], writes=[xq])
                    c.dma('sp', lambda e, xq=xq, r0=r0, db=db: e.dma_start(out=x1_scr.a[r0:r0 + 128, db * 512:(db + 1) * 512], in_=xq.a), reads=[xq], writes=[x1_scr])
            c.barrier(skip_pool_dma=(s == 0))
        if "D" not in stages:
            continue
        trickle(10 ** 9)
        AR.reset()
        keysT = AR.get("keysT", [128, 16, 128], F32)
        fnT = AR.get("fnT", [128, 16, MB], F32)
        mark = AR.off
        kraw = AR.get("kraw", [128, 16, 128], F32)
        c.dma('sp', lambda e: e.dma_start(out=kraw.a, in_=keys.a.rearrange("(g n) c -> n g c", n=128)), reads=[keys], writes=[kraw])
        for g4 in range(4):
            pt = bank("tr")
            for j in range(4):
                c.op('pe', lambda e, pt=pt, j=j, g4=g4: e.transpose(out=pt.a[:, j * 128:(j + 1) * 128], in_=kraw.a[:, g4 * 4 + j, :], identity=ident), reads=[kraw, cst], writes=[pt])
            c.op('dve', lambda e, pt=pt, g4=g4: e.tensor_copy(out=keysT.a[:, g4 * 4:(g4 + 1) * 4, :], in_=pt.a[:, 0:512].rearrange("p (a b) -> p a b", a=4)), reads=[pt], writes=[keysT])
        c.barrier()
        for mbk in range(NMB):
            AR.off = mark
            xt = AR.get("xtD", [128, D], F32); xs2 = AR.get("xs2", [128, D], F32); st = AR.get("stD", [128, 4], F32)
            fnt = AR.get("fnt", [128, D], BF16)
            for tl in range(MB // 128):
                t = mbk * (MB // 128) + tl
                r0 = tok0 + t * 128
                c.dma('sp', lambda e, r0=r0: e.dma_start(out=xt.a, in_=x1_scr.a[r0:r0 + 128, :]), reads=[x1_scr], writes=[xt])
                c.op('dve', lambda e: e.memset(st.a[:, 0:1], 0.0), writes=[st])
                c.op('act', lambda e: e.activation(out=xs2.a, in_=xt.a, func=AF.Square, accum_out=st.a[:, 0:1]), reads=[xt, st], writes=[xs2, st])
                c.op('dve', lambda e: e.tensor_scalar(out=st.a[:, 1:2], in0=st.a[:, 0:1], scalar1=1.0 / D, scalar2=EPS, op0=ALU.mult, op1=ALU.add), reads=[st], writes=[st])
                c.op('act', lambda e: e.activation(out=st.a[:, 2:3], in_=st.a[:, 1:2], func=AF.Sqrt), reads=[st], writes=[st])
                c.op('dve', lambda e: e.reciprocal(out=st.a[:, 3:4], in_=st.a[:, 2:3]), reads=[st], writes=[st])
                c.op('dve', lambda e: e.tensor_scalar(out=xs2.a, in0=xt.a, scalar1=st.a[:, 3:4], scalar2=None, op0=ALU.mult), reads=[xt, st], writes=[xs2])
                for q4 in range(4):
                    pt = bank("tr")
                    for j in range(4):
                        kc = q4 * 4 + j
                        c.op('pe', lambda e, pt=pt, j=j, kc=kc: e.transpose(out=pt.a[:, j * 128:(j + 1) * 128], in_=xs2.a[:, kc * 128:(kc + 1) * 128], identity=ident), reads=[xs2, cst], writes=[pt])
                    for j in range(4):
                        kc = q4 * 4 + j
                        c.op('act', lambda e, pt=pt, j=j, kc=kc, tl=tl: e.activation(out=fnT.a[:, kc, tl * 128:(tl + 1) * 128], in_=pt.a[:, j * 128:(j + 1) * 128], func=AF.Identity,
                             scale=AB.a[:, 2, kc:kc + 1], bias=AB.a[:, 3, kc:kc + 1]), reads=[pt, AB], writes=[fnT])
                for q4 in range(4):
                    pt = bank("tr")
                    for j in range(4):
                        kc = q4 * 4 + j
                        c.op('pe', lambda e, pt=pt, j=j, kc=kc, tl=tl: e.transpose(out=pt.a[:, j * 128:(j + 1) * 128], in_=fnT.a[:, kc, tl * 128:(tl + 1) * 128], identity=ident), reads=[fnT, cst], writes=[pt])
                    c.op('act', lambda e, pt=pt, q4=q4: e.activation(out=fnt.a[:, q4 * 512:(q4 + 1) * 512], in_=pt.a[:, 0:512], func=AF.Copy), reads=[pt], writes=[fnt])
                c.dma('sp', lambda e, t=t: e.dma_start(out=fn_scr.a[t * 128:(t + 1) * 128, :], in_=fnt.a), reads=[fnt], writes=[fn_scr])
            c.barrier()
            AR.off = mark
            wqb = [AR.get("wq%d" % i, [128, 16, 512], F32) for i in range(2)]
            qTc = [AR.get("qTc%d" % i, [128, MB], F32) for i in range(2)]
            s_hp = [AR.get("s_hp%d" % i, [128, MB // 128, 128], F32) for i in range(2)]
            ntl = MB // 128
            for wbk in range(4):
                wq_ = wqb[wbk % 2]
                c.dma('sp', lambda e, wq_=wq_, wbk=wbk: e.dma_start(out=wq_.a, in_=wq.a[:, wbk * 512:(wbk + 1) * 512].rearrange("(k p) n -> p k n", p=128)), reads=[wq], writes=[wq_])
                for j in range(4):
                    hp = wbk * 4 + j
                    qc = qTc[hp % 2]; sh_ = s_hp[hp % 2]
                    ps = bank("proj")
                    for kc in range(16):
                        c.op('pe', lambda e, ps=ps, kc=kc, j=j, wq_=wq_: e.matmul(ps.a[:, 0:MB], lhsT=wq_.a[:, kc, j * 128:(j + 1) * 128], rhs=fnT.a[:, kc, :],
                             start=(kc == 0), stop=(kc == 15)), reads=[wq_, fnT], writes=[ps])
                    c.op('act', lambda e, ps=ps, qc=qc: e.activation(out=qc.a, in_=ps.a[:, 0:MB], func=AF.Copy), reads=[ps], writes=[qc])
                    pss = bank("acc")
                    for tl in range(ntl):
                        c.op('pe', lambda e, pss=pss, tl=tl, qc=qc, hp=hp: e.matmul(pss.a[:, tl * 128:(tl + 1) * 128], lhsT=qc.a[:, tl * 128:(tl + 1) * 128], rhs=keysT.a[:, hp, :],
                             start=True, stop=True), reads=[qc, keysT], writes=[pss])
                    c.op('dve', lambda e, pss=pss, sh_=sh_: e.tensor_copy(out=sh_.a, in_=pss.a[:, 0:ntl * 128].rearrange("p (a b) -> p a b", a=ntl)), reads=[pss], writes=[sh_])
                    c.dma('sp', lambda e, sh_=sh_, hp=hp, mbk=mbk: e.dma_start(out=s_scr.a[mbk * MB:(mbk + 1) * MB, hp * 128:(hp + 1) * 128].rearrange("(t p) n -> p t n", p=128), in_=sh_.a),
                          reads=[sh_], writes=[s_scr])
            c.barrier()
        AR.reset()
        sT_ = AR.get("sD", [128, 16, 128], F32); sW = AR.get("sW", [128, 16, 128], F32)
        x1 = AR.get("x1", [128, D], F32)
        GK = 4
        dg = [AR.get("dg%d" % i, [128, GK, 128], BF16) for i in range(2)]
        tmpD = AR.get("tmpD", [128, 512], F32)
        g2b = AR.get("g2b", [128, D], F32); fgb = AR.get("fgb", [128, D], F32)
        gat = [AR.get("gat%d" % i, [128, 2 * D], BF16) for i in range(5)]
        if BLK >= 1024:
            gat += [Buf("gath%d" % i, hn_raw.ap()[:, i * 4096:(i + 1) * 4096]) for i in range(4)]
        NG = len(gat)
        tops = AR.get("tops", [128, 16, 16], F32); topi = AR.get("topi", [128, 16, 16], U32); topf = AR.get("topf", [128, 16, 16], F32)
        cand = AR.get("cand", [128, 8, 256], F32); candw = AR.get("candw", [128, 8, 256], F32)
        bs = AR.get("bs", [128, 8, 16], F32); bp = AR.get("bp", [128, 8, 16], U32); r01 = AR.get("r01", [128, 2, 8, 16], U32); r01f = AR.get("r01f", [128, 2, 8, 16], F32)
        ij = AR.get("ij", [128, 2, 8, 16], F32)
        oh = candw; oh4 = candw.a.rearrange("p h (a b) -> p h a b", a=16)
        junk = sW; junk2 = sW.a.rearrange("p g n -> p (g n)")
        ef = AR.get("ef", [128, 128], F32); eidx = AR.get("eidx", [128, 128], U32)
        gts = AR.get("gts", [128, 8, 16], F32); gsum = AR.get("gsum", [128, 8], F32)
        pre = AR.get("pre", [128, 128], F32); ge = AR.get("ge", [128, 4, 128], F32); wgt = AR.get("wgt", [128, 128], F32)
        stD = AR.get("stD2", [128, 4], F32)
        fnb = AR.get("fnb", [128, D], BF16)
        junkb = sW.a.rearrange("p g n -> p (g n)").bitcast(BF16)[:, 0:D]
        iota = CS("iota")
        c.dma('sp', lambda e: e.dma_start(out=g2b.a, in_=g2row.a[0:1, :].partition_broadcast(128)), reads=[g2row], writes=[g2b])
        c.dma('sp', lambda e: e.dma_start(out=fgb.a, in_=fgain.a[0:1, :].partition_broadcast(128)), reads=[fgain], writes=[fgb])
        def top16(src, work, nvals, vout, iout):
            c.op('dve', lambda e: e.max(out=vout[1][:, 0:8], in_=src[1]), reads=[src[0]], writes=[vout[0]])
            c.op('dve', lambda e: e.max_index(out=iout[1][:, 0:8], in_max=vout[1][:, 0:8], in_values=src[1]), reads=[src[0], vout[0]], writes=[iout[0]])
            c.op('dve', lambda e: e.match_replace(out=work[1], in_to_replace=vout[1][:, 0:8], in_values=src[1], imm_value=-1e30), reads=[src[0], vout[0]], writes=[work[0]])
            c.op('dve', lambda e: e.max(out=vout[1][:, 8:16], in_=work[1]), reads=[work[0]], writes=[vout[0]])
            c.op('dve', lambda e: e.max_index(out=iout[1][:, 8:16], in_max=vout[1][:, 8:16], in_values=work[1]), reads=[work[0], vout[0]], writes=[iout[0]])
        for t in range(NT):
            r0 = tok0 + t * 128
            c.dma('sp', lambda e, t=t: e.dma_start(out=sT_.a, in_=s_scr.a[t * 128:(t + 1) * 128, :].rearrange("p (g n) -> p g n", g=16)), reads=[s_scr], writes=[sT_])
            c.dma('sp', lambda e, r0=r0: e.dma_start(out=x1.a, in_=x1_scr.a[r0:r0 + 128, :]), reads=[x1_scr], writes=[x1])
            c.dma('sp', lambda e, t=t: e.dma_start(out=fnb.a, in_=fn_scr.a[t * 128:(t + 1) * 128, :]), reads=[fn_scr], writes=[fnb])
            for g in range(16):
                top16((sT_, sT_.a[:, g, :]), (sW, sW.a[:, g, :]), 128, (tops, tops.a[:, g, :]), (topi, topi.a[:, g, :]))
            c.op('dve', lambda e: e.tensor_copy(out=topf.a, in_=topi.a), reads=[topi], writes=[topf])
            t4 = tops.a.rearrange("p (h two) r -> p h two r", two=2)
            c.op('dve', lambda e: e.tensor_tensor(out=cand.a.rearrange("p h (a b) -> p h a b", a=16), in0=t4[:, :, 0, :].unsqueeze(3).to_broadcast([128, 8, 16, 16]),
                 in1=t4[:, :, 1, :].unsqueeze(2).to_broadcast([128, 8, 16, 16]), op=ALU.add), reads=[tops], writes=[cand])
            for h in range(8):
                top16((cand, cand.a[:, h, :]), (candw, candw.a[:, h, :]), 256, (bs, bs.a[:, h, :]), (bp, bp.a[:, h, :]))
            c.op('dve', lambda e: e.tensor_single_scalar(out=r01.a[:, 0], in_=bp.a, scalar=4, op=ALU.logical_shift_right), reads=[bp], writes=[r01])
            c.op('dve', lambda e: e.tensor_single_scalar(out=r01.a[:, 1], in_=bp.a, scalar=15, op=ALU.bitwise_and), reads=[bp], writes=[r01])
            c.op('dve', lambda e: e.tensor_copy(out=r01f.a, in_=r01.a), reads=[r01], writes=[r01f])
            f4 = topf.a.rearrange("p (h two) r -> p h two r", two=2)
            for side in range(2):
                c.op('dve', lambda e, side=side: e.tensor_tensor(out=oh4, in0=r01f.a[:, side].unsqueeze(3).to_broadcast([128, 8, 16, 16]),
                     in1=iota.unsqueeze(1).unsqueeze(1).to_broadcast([128, 8, 16, 16]), op=ALU.is_equal), reads=[r01f, cst], writes=[oh])
                c.op('dve', lambda e, side=side: e.tensor_tensor(out=oh4, in0=oh4, in1=f4[:, :, side, :].unsqueeze(2).to_broadcast([128, 8, 16, 16]), op=ALU.mult), reads=[oh, topf], writes=[oh])
                c.op('dve', lambda e, side=side: e.tensor_reduce(out=ij.a[:, side], in_=oh4, axis=AX.X, op=ALU.add), reads=[oh], writes=[ij])
            c.op('dve', lambda e: e.scalar_tensor_tensor(out=ef.a.rearrange("p (h r) -> p h r", h=8), in0=ij.a[:, 0], scalar=128.0, in1=ij.a[:, 1], op0=ALU.mult, op1=ALU.add), reads=[ij], writes=[ef])
            c.op('dve', lambda e: e.tensor_copy(out=eidx.a, in_=ef.a), reads=[ef], writes=[eidx])
            c.op('dve', lambda e: e.tensor_tensor(out=gts.a, in0=bs.a, in1=bs.a[:, :, 0:1].to_broadcast([128, 8, 16]), op=ALU.subtract), reads=[bs], writes=[gts])
            c.op('act', lambda e: e.activation(out=gts.a, in_=gts.a, func=AF.Exp), reads=[gts], writes=[gts])
            c.op('dve', lambda e: e.tensor_reduce(out=gsum.a, in_=gts.a, axis=AX.X, op=ALU.add), reads=[gts], writes=[gsum])
            c.op('dve', lambda e: e.reciprocal(out=gsum.a, in_=gsum.a), reads=[gsum], writes=[gsum])
            c.op('dve', lambda e: e.tensor_tensor(out=gts.a, in0=gts.a, in1=gsum.a.unsqueeze(2).to_broadcast([128, 8, 16]), op=ALU.mult), reads=[gts, gsum], writes=[gts])
            PY = [pb[i] for i in range(4)]
            c.op('dve', lambda e: e.memset(pre.a, 0.0), writes=[pre])
            for grp in range(128 // GK):
                k0 = grp * GK
                sl = slice(k0, k0 + GK)
                for j in range(GK):
                    k = k0 + j
                    gb = gat[k % NG]
                    c.dma('pool', lambda e, gb=gb, k=k: e.indirect_dma_start(out=gb.a, out_offset=None, in_=uv_bf.a, in_offset=bass.IndirectOffsetOnAxis(ap=eidx.a[:, k:k + 1], axis=0)),
                          reads=[eidx, uv_bf], writes=[gb])
                    c.op('dve', lambda e, gb=gb, k=k: e.scalar_tensor_tensor(out=junkb, in0=gb.a[:, 0:D], scalar=1.0, in1=fnb.a, op0=ALU.mult, op1=ALU.mult, accum_out=pre.a[:, k:k + 1]),
                         reads=[gb, fnb, pre], writes=[junk, pre])
                c.op('dve', lambda e, sl=sl: e.tensor_tensor(out=ge.a[:, 0, sl], in0=pre.a[:, sl], in1=pre.a[:, sl], op=ALU.mult), reads=[pre], writes=[ge])
                c.op('dve', lambda e, sl=sl: e.tensor_scalar(out=ge.a[:, 1, sl], in0=ge.a[:, 0, sl], scalar1=0.044715, scalar2=1.0, op0=ALU.mult, op1=ALU.add), reads=[ge], writes=[ge])
                c.op('dve', lambda e, sl=sl: e.tensor_tensor(out=ge.a[:, 2, sl], in0=ge.a[:, 1, sl], in1=pre.a[:, sl], op=ALU.mult), reads=[ge, pre], writes=[ge])
                c.op('act', lambda e, sl=sl: e.activation(out=ge.a[:, 3, sl], in_=ge.a[:, 2, sl], func=AF.Sigmoid, scale=1.5957691216057308), reads=[ge], writes=[ge])
                c.op('dve', lambda e, sl=sl: e.tensor_tensor(out=wgt.a[:, sl], in0=ge.a[:, 3, sl], in1=pre.a[:, sl], op=ALU.mult), reads=[ge, pre], writes=[wgt])
                c.op('dve', lambda e, sl=sl: e.tensor_tensor(out=wgt.a[:, sl], in0=wgt.a[:, sl], in1=gts.a.rearrange("p h r -> p (h r)")[:, sl], op=ALU.mult), reads=[wgt, gts], writes=[wgt])
                dgk = dg[grp % 2]
                for j in range(GK):
                    c.op('act', lambda e, dgk=dgk, j=j, k0=k0: e.activation(out=dgk.a[:, j, :], in_=identb.a, func=AF.Copy, scale=wgt.a[:, k0 + j:k0 + j + 1]),
                         reads=[identb, wgt], writes=[dgk])
                for j in range(GK):
                    k = k0 + j
                    gb = gat[k % NG]
                    for nb in range(4):
                        c.op('pe', lambda e, gb=gb, k=k, nb=nb, dgk=dgk, j=j: e.matmul(PY[nb].a[:, 0:512], lhsT=dgk.a[:, j, :], rhs=gb.a[:, D + nb * 512:D + (nb + 1) * 512],
                             start=(k == 0), stop=(k == 127)), reads=[dgk, gb], writes=[PY[nb]])
            for nb in range(4):
                c.op('dve', lambda e, nb=nb: e.tensor_tensor(out=tmpD.a, in0=PY[nb].a[:, 0:512], in1=g2b.a[:, nb * 512:(nb + 1) * 512], op=ALU.mult), reads=[PY[nb], g2b], writes=[tmpD])
                c.op('dve', lambda e, nb=nb: e.tensor_tensor(out=x1.a[:, nb * 512:(nb + 1) * 512], in0=x1.a[:, nb * 512:(nb + 1) * 512], in1=tmpD.a, op=ALU.add), reads=[x1, tmpD], writes=[x1])
            c.op('dve', lambda e: e.memset(stD.a[:, 0:1], 0.0), writes=[stD])
            c.op('act', lambda e: e.activation(out=junk2, in_=x1.a, func=AF.Square, accum_out=stD.a[:, 0:1]), reads=[x1, stD], writes=[junk, stD])
            c.op('dve', lambda e: e.tensor_scalar(out=stD.a[:, 1:2], in0=stD.a[:, 0:1], scalar1=1.0 / D, scalar2=EPS, op0=ALU.mult, op1=ALU.add), reads=[stD], writes=[stD])
            c.op('act', lambda e: e.activation(out=stD.a[:, 2:3], in_=stD.a[:, 1:2], func=AF.Sqrt), reads=[stD], writes=[stD])
            c.op('dve', lambda e: e.reciprocal(out=stD.a[:, 3:4], in_=stD.a[:, 2:3]), reads=[stD], writes=[stD])
            c.op('dve', lambda e: e.scalar_tensor_tensor(out=x1.a, in0=x1.a, scalar=stD.a[:, 3:4], in1=fgb.a, op0=ALU.mult, op1=ALU.mult), reads=[x1, stD, fgb], writes=[x1])
            c.dma('sp', lambda e, r0=r0: e.dma_start(out=out.a[r0:r0 + 128, :], in_=x1.a), reads=[x1], writes=[out])
        c.barrier()
    c.finish()
    return nc, c, lay, NCONST


def _consts(core, BLK, NT, NPRE, lay, NCONST, TPC):
    cs = np.zeros((128, NCONST), np.float32)
    def put(nm, arr):
        o, w = lay[nm]
        cs[:, o:o + w] = arr
    put("ident", np.eye(128, dtype=np.float32))
    g = np.array(GAM, np.float64)
    i = np.arange(128)
    diff = i[None, :] - i[:, None]
    dm = np.zeros((128, 8, 128))
    for h in range(8):
        dm[:, h, :] = np.where(diff >= 0, g[h] ** np.maximum(diff, 0), 0.0) / 16.0
    put("dmaskT", dm.reshape(128, 1024))
    qd = np.stack([g[h] ** (i + 1.0) for h in range(8)], 0)
    put("qdec", np.broadcast_to(qd.reshape(1, 1024), (128, 1024)))
    put("kdown", np.stack([g[h] ** (127.0 - i) / 16.0 for h in range(8)], 1))
    kp = np.zeros((128, 8, NT))
    for h in range(8):
        for n in range(NT):
            kp[:, h, n] = g[h] ** (BLK - 1.0 - (128 * n + i)) / 16.0
    put("kdpre", kp.reshape(128, 8 * NT))
    own_start = core * TPC
    co = np.zeros((NPRE, 8))
    for p in range(NPRE):
        if own_start - (NPRE - p) * BLK >= 0:
            co[p] = g ** (BLK * (NPRE - 1.0 - p))
    put("coef", np.broadcast_to(co.reshape(1, NPRE * 8), (128, NPRE * 8)))
    invf = (np.float32(10000.0) ** (-(np.arange(0, 256, 2, dtype=np.float32)) / np.float32(256.0))).astype(np.float32)
    put("invf", invf.reshape(128, 1))
    put("hm", np.full((128, 1), 0.0 if core == 0 else 1.0))
    pc = np.ones((4, 16))
    if core == 0:
        for gi, w in enumerate(POOLW):
            pc[gi] = w / np.minimum(np.arange(16) + 1.0, w)
    put("pcorr", np.broadcast_to(pc.reshape(1, 64), (128, 64)))
    put("iota", np.broadcast_to(np.arange(16, dtype=np.float32).reshape(1, 16), (128, 16)))
    return cs


_CACHE = {}


def _run(inputs, SEQ, BLK, stages="ABCD", trace=False):
    f = lambda k: np.ascontiguousarray(np.asarray(inputs[k]))
    TPC = SEQ // NCORES
    NT = BLK // 128
    NPRE = 7 * (TPC // BLK)
    key = (SEQ, BLK, stages)
    if key not in _CACHE:
        _CACHE[key] = build_nc(SEQ, BLK, stages)
    nc, ctx, lay, NCONST = _CACHE[key]
    x = f("x")[0]
    pos = f("positions")[0].astype(np.int32)
    pvec1 = np.concatenate([f("b_ada")[0].reshape(96, 128), f("norm_mix_gain")[0].reshape(16, 128), f("norm_ffn_gain")[0].reshape(16, 128)], 0).astype(np.float32)
    pvec2 = np.concatenate([f("pool_scale")[0].reshape(16, 128), f("ret_norm_gain")[0].reshape(32, 128), f("c")[0].reshape(16, 128), np.zeros((64, 128), np.float32)], 0).astype(np.float32)
    shared = {
        "pvec1": pvec1, "pvec2": pvec2, "b_ada": f("b_ada")[0].reshape(1, -1), "fgain": f("final_norm_gain").reshape(1, -1),
        "w_ada": f("w_ada")[0], "w_in": f("w_in")[0], "pool_w": f("pool_w")[0].reshape(2048, 512),
        "wbp": f("w_branch_pool")[0], "wbr": f("w_branch_ret")[0], "wout": f("w_out")[0], "wq": f("peer_w_query")[0],
        "keys": f("peer_sub_keys")[0].reshape(2048, 128), "peer_u": f("peer_u")[0], "peer_v": f("peer_v")[0],
    }
    in_maps = []
    for core in range(NCORES):
        s0 = core * TPC
        npre = NPRE * BLK
        xp = np.zeros((npre, D), np.float32)
        pp = np.zeros((1, npre), np.int32)
        lo = s0 - npre
        if lo < 0:
            if s0 > 0:
                xp[-s0:] = x[0:s0]
                pp[0, -s0:] = pos[0:s0]
        else:
            xp[:] = x[lo:s0]
            pp[0, :] = pos[lo:s0]
        m = dict(shared)
        m["x_own"] = np.ascontiguousarray(x[s0:s0 + TPC])
        m["x_pre"] = xp
        m["pos_own"] = np.ascontiguousarray(pos[s0:s0 + TPC]).reshape(1, TPC)
        m["pos_pre"] = pp
        m["consts"] = _consts(core, BLK, NT, NPRE, lay, NCONST, TPC)
        in_maps.append(m)
    res = run_bass_kernel_spmd(nc, in_maps, core_ids=list(range(NCORES)), trace=trace)
    outs = [np.asarray(r["out"]) for r in res.results]
    return np.concatenate(outs, 0).reshape(1, SEQ, D).astype(np.float32), res


def kernel(**inputs):
    out, _ = _run(inputs, 16384, 1024)
    return out
```

```python
import numpy as np
import concourse.bass as bass
import concourse.mybir as mybir
from concourse.bass_utils import run_bass_kernel_spmd

F32 = mybir.dt.float32
BF16 = mybir.dt.bfloat16
I32 = mybir.dt.int32
U32 = mybir.dt.uint32
ALU = mybir.AluOpType
AF = mybir.ActivationFunctionType
AX = mybir.AxisListType

NCORES = 8
D = 2048
SEQ = 16384
TPC = SEQ // NCORES
NRING = 8


class Buf:
    def __init__(self, name, ap=None):
        self.name = name
        self.a = ap
        self.lw = None
        self.rd = {}

    def __getitem__(self, k):
        return self.a[k]


class Ctx:
    def __init__(self, nc, same_eng_sync=True):
        self.nc = nc
        self.eng = {'pe': nc.tensor, 'act': nc.scalar, 'dve': nc.vector, 'pool': nc.gpsimd, 'sp': nc.sync}
        self.cnt = {e: 0 for e in ('pe', 'act', 'dve', 'pool')}
        self.csem = {e: nc.alloc_semaphore('c_' + e) for e in ('pe', 'act', 'dve', 'pool')}
        self.ring = {q: [nc.alloc_semaphore('r_%s_%d' % (q, i)) for i in range(NRING)] for q in ('sp', 'act', 'pool')}
        self.dcnt = {q: 0 for q in ('sp', 'act', 'pool')}
        self.seen = {e: {} for e in self.eng}
        self.same = same_eng_sync
        self.nid = 0
        self.nwaits = 0
        self.ninstr = 0

    def sb(self, name, shape, dt=F32):
        self.nid += 1
        t = self.nc.alloc_sbuf_tensor('%s_%d' % (name, self.nid), list(shape), dt)
        return Buf(name, t.ap())

    def ps(self, name, shape, dt=F32):
        self.nid += 1
        t = self.nc.alloc_psum_tensor('%s_%d' % (name, self.nid), list(shape), dt)
        return Buf(name, t.ap())

    def dram(self, name, shape, dt=F32, kind="Internal"):
        t = self.nc.dram_tensor(name, list(shape), dt, kind=kind)
        return Buf(name, t.ap())

    def _sem(self, key):
        return self.csem[key[1]] if key[0] == 'c' else self.ring[key[1]][key[2]]

    def _wait(self, E, deps):
        seen = self.seen[E]
        for key, val in deps:
            if key[0] == 'c' and key[1] == E and (E == 'pe' or not self.same):
                continue
            if seen.get(key, 0) >= val:
                continue
            self.eng[E].wait_ge(self._sem(key), val)
            seen[key] = val
            self.nwaits += 1

    def _deps(self, reads, writes):
        deps = []
        for b in reads:
            if b.lw is not None:
                deps.append(b.lw)
        for b in writes:
            if b.lw is not None:
                deps.append(b.lw)
            deps.extend(b.rd.items())
        return deps

    def _commit(self, ev, reads, writes):
        key, val = ev
        for b in reads:
            if b.rd.get(key, 0) < val:
                b.rd[key] = val
        for b in writes:
            b.lw = ev
            b.rd = {}

    def op(self, E, fn, reads=(), writes=()):
        self._wait(E, self._deps(reads, writes))
        ins = fn(self.eng[E])
        self.cnt[E] += 1
        ins.then_inc(self.csem[E], 1)
        self.ninstr += 1
        self._commit((('c', E), self.cnt[E]), reads, writes)
        return ins

    def dma(self, q, fn, reads=(), writes=()):
        k = self.dcnt[q]
        s = k % NRING
        gen = k // NRING
        deps = self._deps(reads, writes)
        if gen > 0:
            deps.append((('d', q, s), 16 * gen))
        self._wait(q, deps)
        ins = fn(self.eng[q])
        ins.then_inc(self.ring[q][s], 16)
        self.dcnt[q] = k + 1
        self.ninstr += 1
        self._commit((('d', q, s), 16 * (gen + 1)), reads, writes)
        return ins

    def finish(self):
        for q in ('sp', 'act', 'pool'):
            k = self.dcnt[q]
            deps = []
            for s in range(NRING):
                n = (k - s + NRING - 1) // NRING
                if n > 0:
                    deps.append((('d', q, s), 16 * n))
            self._wait(q, deps)

    def barrier(self, skip_pool_dma=False):
        deps = [((('c', e)), self.cnt[e]) for e in self.cnt if self.cnt[e] > 0]
        for q in (('sp', 'act') if skip_pool_dma else ('sp', 'act', 'pool')):
            k = self.dcnt[q]
            for s in range(NRING):
                n = (k - s + NRING - 1) // NRING
                if n > 0:
                    deps.append((('d', q, s), 16 * n))
        for E in self.eng:
            self._wait(E, deps)


import math
PI = math.pi
HEADS = 8
DK = 256
DV = 512
GAM = [1.0 - 2.0 ** (-5.0 - h) for h in range(HEADS)]
POOLW = (2, 4, 8, 16)
HALO = 16
NGAT = 2
PREFIX_SKIP = 2.0 ** -40
EPS = 1e-6
NEXP = 16384
C_U, C_Q, C_K, C_V, C_G, C_AP, C_AR = 0, 2048, 4096, 6144, 10240, 14336, 16384
WIN_COLS = 18432


class Arena:
    def __init__(self, c, nbytes):
        self.c = c
        self.n = nbytes // 2
        c.nid += 1
        self.t = c.nc.alloc_sbuf_tensor('arena_%d' % c.nid, [128, self.n], BF16)
        self.off = 0

    def reset(self):
        self.off = 0

    def get(self, name, shape, dt=BF16):
        free = 1
        for s in shape[1:]:
            free *= s
        esz = 4 if dt in (F32, I32, U32) else 2
        nel2 = free * esz // 2
        self.off = (self.off + 1) // 2 * 2
        assert self.off + nel2 <= self.n, ("arena overflow", name, self.off, nel2, self.n)
        ap = self.t.ap()[0:shape[0], self.off:self.off + nel2]
        self.off += nel2
        if dt != BF16:
            ap = ap.bitcast(dt)
        if len(shape) == 3:
            ap = ap.rearrange("p (a b) -> p a b", a=shape[1])
        elif len(shape) == 4:
            ap = ap.rearrange("p (a b c) -> p a b c", a=shape[1], b=shape[2])
        return Buf(name, ap)


def const_layout(NT, NPRE):
    lay = {}
    o = 0
    for nm, w in (("ident", 128), ("dmaskT", 8 * 128), ("qdec", 8 * 128), ("kdown", 8), ("kdpre", 8 * NT),
                  ("coef", NPRE * 8), ("invf", 1), ("hm", 1), ("pcorr", 64), ("iota", 16)):
        lay[nm] = (o, w)
        o += w
    return lay, o


def build_nc(SEQ=16384, BLK=1024, stages="ABCD"):
    TPC = SEQ // NCORES
    NSUB = TPC // BLK
    NPRE = 7 * NSUB
    NT = BLK // 128
    MB = min(512, BLK)
    NMB = BLK // MB
    nc = bass.Bass("TRN2", target_bir_lowering=False)
    c = Ctx(nc)
    lay, NCONST = const_layout(NT, NPRE)

    def din(name, shape, dt=F32):
        return c.dram(name, shape, dt, kind="ExternalInput")

    x_own = din("x_own", [TPC, D]); x_pre = din("x_pre", [NPRE * BLK, D])
    pos_own = din("pos_own", [1, TPC], I32); pos_pre = din("pos_pre", [1, NPRE * BLK], I32)
    pvec1 = din("pvec1", [128, 128]); pvec2 = din("pvec2", [128, 128])
    b_ada = din("b_ada", [1, 6 * D]); fgain = din("fgain", [1, D])
    w_ada = din("w_ada", [D, 6 * D]); w_in = din("w_in", [D, WIN_COLS])
    pool_w = din("pool_w", [4 * 512, 512]); wbp = din("wbp", [D, D]); wbr = din("wbr", [2 * D, D]); wout = din("wout", [D, D])
    wq = din("wq", [D, D]); keys = din("keys", [16 * 128, 128])
    peer_u = din("peer_u", [NEXP, D]); peer_v = din("peer_v", [NEXP, D])
    consts_d = din("consts", [128, NCONST])
    out = c.dram("out", [TPC, D], F32, kind="ExternalOutput")
    win_b = c.dram("win_b", [D, WIN_COLS], BF16)
    poolw_b = c.dram("poolw_b", [4 * 512, 512], BF16)
    wbp_b = c.dram("wbp_b", [D, D], BF16); wbr_b = c.dram("wbr_b", [2 * D, D], BF16); wout_b = c.dram("wout_b", [D, D], BF16)
    ret_scr = c.dram("ret_scr", [32 * 128, TPC], BF16)
    x1_scr = c.dram("x1_scr", [TPC, D], F32)
    s_scr = c.dram("s_scr", [BLK, D], F32)
    g2row = c.dram("g2row", [1, D], F32)
    uv_bf = c.dram("uv_bf", [NEXP, 2 * D], BF16)
    fn_scr = c.dram("fn_scr", [BLK, D], BF16)

    cst = c.sb("consts", [128, NCONST])
    def CS(nm):
        o, w = lay[nm]
        return cst.a[:, o:o + w]
    ident = CS("ident")
    identb = c.sb("identb", [128, 128], BF16)
    vecT1 = c.sb("vecT1", [128, 128]); vecT2 = c.sb("vecT2", [128, 128])
    AB = c.sb("AB", [128, 4, 16])
    g1bc = c.sb("g1bc", [128, D])
    Sst = [c.sb("S%d" % h, [128, 2, 512]) for h in range(HEADS)]
    hn_raw = c.nc.alloc_sbuf_tensor("hn_raw", [128, 16 * (HALO + BLK)], BF16)
    hnT = Buf("hnT", hn_raw.ap().rearrange("p (k t) -> p k t", k=16))
    halo_save = c.sb("halo_save", [128, 16, HALO], BF16)
    small = c.sb("small", [128, 16])
    pb = [c.ps("pb%d" % i, [128, 512]) for i in range(8)]
    rr = {"proj": 0, "acc": 0, "tr": 0}
    def bank(role):
        base, n = {"proj": (0, 4), "acc": (4, 2), "tr": (6, 2)}[role]
        i = rr[role]; rr[role] = (i + 1) % n
        return pb[base + i]
    AR = Arena(c, 120 * 1024 + 512)

    ld = lambda dst, src, rd, q='sp': c.dma(q, lambda e: e.dma_start(out=dst.a if isinstance(dst, Buf) else dst[1], in_=src.a if isinstance(src, Buf) else src[1]),
                                            reads=[src if isinstance(src, Buf) else src[0]], writes=[dst if isinstance(dst, Buf) else dst[0]])

    ld(cst, consts_d, None)
    c.op('dve', lambda e: e.tensor_copy(out=identb.a, in_=ident), reads=[cst], writes=[identb])
    for h in range(HEADS):
        c.op('pool', lambda e, h=h: e.memset(Sst[h].a, 0.0), writes=[Sst[h]])
    c.op('pool', lambda e: e.memset(halo_save.a, 0.0), writes=[halo_save])
    def cast(dst, src, r0, r1, c0, c1):
        c.dma('pool', lambda e: e.dma_start(out=dst.a[r0:r1, c0:c1], in_=src.a[r0:r1, c0:c1]), reads=[src], writes=[dst])
    for r0 in range(0, D, 512):
        cast(win_b, w_in, r0, r0 + 512, C_K, C_G)

    late = []
    def _piece(dst, src, r0, r1, c0, c1, d0=None):
        d0 = c0 if d0 is None else d0
        late.append(lambda: c.dma('pool', lambda e: e.dma_start(out=dst.a[r0:r1, d0:d0 + (c1 - c0)], in_=src.a[r0:r1, c0:c1]), reads=[src], writes=[dst]))
    for (c0, c1) in ((C_Q, C_K), (C_G, C_AP), (C_U, C_Q), (C_AP, WIN_COLS)):
        for cc0 in range(c0, c1, 2048):
            for r0 in range(0, D, 512):
                _piece(win_b, w_in, r0, r0 + 512, cc0, cc0 + 2048)
    _piece(poolw_b, pool_w, 0, 2048, 0, 512)
    for r0 in range(0, D, 512):
        _piece(wbp_b, wbp, r0, r0 + 512, 0, D)
    for r0 in range(0, 2 * D, 512):
        _piece(wbr_b, wbr, r0, r0 + 512, 0, D)
    for r0 in range(0, D, 512):
        _piece(wout_b, wout, r0, r0 + 512, 0, D)
    n_weight_pieces = len(late)
    if "D" in stages:
        for r0 in range(0, NEXP, 512):
            _piece(uv_bf, peer_u, r0, r0 + 512, 0, D, d0=0)
            _piece(uv_bf, peer_v, r0, r0 + 512, 0, D, d0=D)
    late_i = [0]
    def trickle(n=1, upto=None):
        lim = len(late) if upto is None else min(upto, len(late))
        while n > 0 and late_i[0] < lim:
            late[late_i[0]]()
            late_i[0] += 1
            n -= 1

    AR.reset()
    pv = AR.get("pv", [128, 2, 128], F32)
    c.dma('sp', lambda e: e.dma_start(out=pv.a[:, 0, :], in_=pvec1.a), reads=[pvec1], writes=[pv])
    c.dma('sp', lambda e: e.dma_start(out=pv.a[:, 1, :], in_=pvec2.a), reads=[pvec2], writes=[pv])
    for j, vt in enumerate((vecT1, vecT2)):
        pt = bank("tr")
        c.op('pe', lambda e, j=j, pt=pt: e.transpose(out=pt.a[:, 0:128], in_=pv.a[:, j, :], identity=ident), reads=[pv, cst], writes=[pt])
        c.op('dve', lambda e, vt=vt, pt=pt: e.tensor_copy(out=vt.a, in_=pt.a[:, 0:128]), reads=[pt], writes=[vt])
    cond = AR.get("cond", [128, 16], F32)
    c.op('act', lambda e: e.activation(out=cond.a, in_=vecT2.a[:, 48:64], func=AF.Silu), reads=[vecT2], writes=[cond])
    condB = AR.get("condB", [128, 16, 128], F32)
    c.op('dve', lambda e: e.tensor_copy(out=condB.a, in_=cond.a.unsqueeze(2).to_broadcast([128, 16, 128])), reads=[cond], writes=[condB])
    g2bc = AR.get("g2bc", [128, D], F32)
    c.dma('sp', lambda e: e.dma_start(out=g1bc.a, in_=b_ada.a[0:1, 2 * D:3 * D].partition_broadcast(128)), reads=[b_ada], writes=[g1bc])
    c.dma('sp', lambda e: e.dma_start(out=g2bc.a, in_=b_ada.a[0:1, 5 * D:6 * D].partition_broadcast(128)), reads=[b_ada], writes=[g2bc])
    wab = [AR.get("wab%d" % i, [128, 16, 512], F32) for i in range(3)]
    modT = AR.get("modT", [128, 64], F32)
    pm = bank("acc")
    fm_blocks = {0: 0, 1: 1, 2: 2, 3: 3, 4: 4, 5: 5, 6: 6, 7: 7, 12: 8, 13: 9, 14: 10, 15: 11, 16: 12, 17: 13, 18: 14, 19: 15}
    for b in range(24):
        wb_ = wab[b % 3]
        c.dma('sp', lambda e, wb_=wb_, b=b: e.dma_start(out=wb_.a, in_=w_ada.a[:, b * 512:(b + 1) * 512].rearrange("(k p) n -> p k n", p=128)),
              reads=[w_ada], writes=[wb_])
        if b in fm_blocks:
            for jj in range(4):
                col = fm_blocks[b] * 4 + jj
                for kc in range(16):
                    c.op('pe', lambda e, wb_=wb_, jj=jj, kc=kc, col=col: e.matmul(pm.a[:, col:col + 1], lhsT=wb_.a[:, kc, jj * 128:(jj + 1) * 128],
                         rhs=cond.a[:, kc:kc + 1], start=(kc == 0), stop=(kc == 15)), reads=[wb_, cond], writes=[pm])
        else:
            tgt = g1bc if b < 12 else g2bc
            blk = (b - 8) if b < 12 else (b - 20)
            pg = bank("proj")
            for kc in range(16):
                c.op('pe', lambda e, wb_=wb_, kc=kc, pg=pg: e.matmul(pg.a[:, 0:512], lhsT=condB.a[:, kc, :], rhs=wb_.a[:, kc, :],
                     start=(kc == 0), stop=(kc == 15)), reads=[wb_, condB], writes=[pg])
            c.op('dve', lambda e, tgt=tgt, blk=blk, pg=pg: e.tensor_tensor(out=tgt.a[:, blk * 512:(blk + 1) * 512], in0=pg.a[:, 0:512],
                 in1=tgt.a[:, blk * 512:(blk + 1) * 512], op=ALU.add), reads=[pg, tgt], writes=[tgt])
    c.op('dve', lambda e: e.tensor_tensor(out=modT.a[:, 0:32], in0=pm.a[:, 0:32], in1=vecT1.a[:, 0:32], op=ALU.add), reads=[pm, vecT1], writes=[modT])
    c.op('dve', lambda e: e.tensor_tensor(out=modT.a[:, 32:64], in0=pm.a[:, 32:64], in1=vecT1.a[:, 48:80], op=ALU.add), reads=[pm, vecT1], writes=[modT])
    for i, (sc0, g0, sh0) in enumerate(((16, 96, 0), (48, 112, 32))):
        c.op('dve', lambda e, i=i, sc0=sc0, g0=g0: e.scalar_tensor_tensor(out=AB.a[:, 2 * i, :], in0=modT.a[:, sc0:sc0 + 16], scalar=1.0,
             in1=vecT1.a[:, g0:g0 + 16], op0=ALU.add, op1=ALU.mult), reads=[modT, vecT1], writes=[AB])
        c.op('dve', lambda e, i=i, sh0=sh0: e.tensor_copy(out=AB.a[:, 2 * i + 1, :], in_=modT.a[:, sh0:sh0 + 16]), reads=[modT], writes=[AB])
    c.dma('sp', lambda e: e.dma_start(out=g2row.a, in_=g2bc.a[0:1, :]), reads=[g2bc], writes=[g2row])
    c.barrier(skip_pool_dma=True)

    def alloc_A():
        xtb = [AR.get("xt%d" % i, [128, D], F32) for i in range(2)]
        xsb = [AR.get("xs%d" % i, [128, D], BF16) for i in range(2)]
        junk = AR.get("junk", [128, D], BF16)
        stb = [AR.get("stA%d" % i, [128, 4], F32) for i in range(2)]
        return xtb, xsb, junk, stb

    def stage_A(xsrc, r0, bufs=None):
        if bufs is None:
            AR.reset()
            bufs = alloc_A()
        xtb, xsb, junk, stb = bufs
        c.op('dve', lambda e: e.tensor_copy(out=hnT.a[:, :, 0:HALO], in_=halo_save.a), reads=[halo_save], writes=[hnT])
        for t in range(NT):
            xt = xtb[t % 2]; xs = xsb[t % 2]; st = stb[t % 2]
            c.dma('sp', lambda e, xt=xt, t=t: e.dma_start(out=xt.a, in_=xsrc.a[r0 + t * 128:r0 + (t + 1) * 128, :]), reads=[xsrc], writes=[xt])
            c.op('dve', lambda e: e.memset(st.a[:, 0:1], 0.0), writes=[st])
            c.op('act', lambda e, xt=xt: e.activation(out=junk.a, in_=xt.a, func=AF.Square, accum_out=st.a[:, 0:1]), reads=[xt, st], writes=[junk, st])
            c.op('dve', lambda e: e.tensor_scalar(out=st.a[:, 1:2], in0=st.a[:, 0:1], scalar1=1.0 / D, scalar2=EPS, op0=ALU.mult, op1=ALU.add), reads=[st], writes=[st])
            c.op('act', lambda e: e.activation(out=st.a[:, 2:3], in_=st.a[:, 1:2], func=AF.Sqrt), reads=[st], writes=[st])
            c.op('dve', lambda e: e.reciprocal(out=st.a[:, 3:4], in_=st.a[:, 2:3]), reads=[st], writes=[st])
            c.op('dve', lambda e, xt=xt: e.tensor_scalar(out=xs.a, in0=xt.a, scalar1=st.a[:, 3:4], scalar2=None, op0=ALU.mult), reads=[xt, st], writes=[xs])
            for half in range(2):
                pt = bank("tr"); ptb = pt.a.bitcast(BF16)
                for j in range(8):
                    kc = half * 8 + j
                    c.op('pe', lambda e, kc=kc, j=j, ptb=ptb: e.transpose(out=ptb[:, j * 128:(j + 1) * 128], in_=xs.a[:, kc * 128:(kc + 1) * 128], identity=identb.a),
                         reads=[xs, identb], writes=[pt])
                for j in range(8):
                    kc = half * 8 + j
                    if j % 2 == 0:
                        c.op('act', lambda e, kc=kc, j=j, ptb=ptb, t=t: e.activation(out=hnT.a[:, kc, HALO + t * 128:HALO + (t + 1) * 128], in_=ptb[:, j * 128:(j + 1) * 128],
                             func=AF.Identity, scale=AB.a[:, 0, kc:kc + 1], bias=AB.a[:, 1, kc:kc + 1]), reads=[pt, AB], writes=[hnT])
                    else:
                        c.op('dve', lambda e, kc=kc, j=j, ptb=ptb, t=t: e.tensor_scalar(out=hnT.a[:, kc, HALO + t * 128:HALO + (t + 1) * 128], in0=ptb[:, j * 128:(j + 1) * 128],
                             scalar1=AB.a[:, 0, kc:kc + 1], scalar2=AB.a[:, 1, kc:kc + 1], op0=ALU.mult, op1=ALU.add), reads=[pt, AB], writes=[hnT])

        c.op('dve', lambda e: e.tensor_copy(out=halo_save.a, in_=hnT.a[:, :, BLK:BLK + HALO]), reads=[hnT], writes=[halo_save])

    def rope_tables(psrc, p0, cs, tmp=None):
        C1 = 6.28125; C2 = 2 * PI - 6.28125
        if tmp is None:
            tmp = (AR.get("pi", [128, BLK], I32), AR.get("ang", [128, BLK], F32), AR.get("kf", [128, BLK], F32))
        pi_, ang, kf = tmp
        m = kf
        invf = CS("invf")
        c.dma('sp', lambda e: e.dma_start(out=pi_.a, in_=psrc.a[0:1, p0:p0 + BLK].partition_broadcast(128)), reads=[psrc], writes=[pi_])
        c.op('dve', lambda e: e.tensor_copy(out=kf.a, in_=pi_.a), reads=[pi_], writes=[kf])
        c.op('dve', lambda e: e.tensor_scalar(out=ang.a, in0=kf.a, scalar1=invf, scalar2=None, op0=ALU.mult), reads=[kf, cst], writes=[ang])
        for j, sh in enumerate([0.0, 0.5 * PI]):
            r_ = cs.a[:, j, :]
            c.op('dve', lambda e: e.tensor_scalar(out=kf.a, in0=ang.a, scalar1=sh, scalar2=1.0 / (2 * PI), op0=ALU.add, op1=ALU.mult), reads=[ang], writes=[kf])
            c.op('dve', lambda e: e.tensor_copy(out=pi_.a, in_=kf.a), reads=[kf], writes=[pi_])
            c.op('dve', lambda e: e.tensor_copy(out=kf.a, in_=pi_.a), reads=[pi_], writes=[kf])
            c.op('dve', lambda e, r_=r_: e.scalar_tensor_tensor(out=r_, in0=kf.a, scalar=-C1, in1=ang.a, op0=ALU.mult, op1=ALU.add), reads=[kf, ang], writes=[cs])
            c.op('dve', lambda e, r_=r_: e.scalar_tensor_tensor(out=r_, in0=kf.a, scalar=-C2, in1=r_, op0=ALU.mult, op1=ALU.add), reads=[kf, cs], writes=[cs])
            if sh != 0.0:
                c.op('dve', lambda e, r_=r_: e.tensor_scalar(out=r_, in0=r_, scalar1=sh, scalar2=None, op0=ALU.add), reads=[cs], writes=[cs])
            c.op('dve', lambda e, r_=r_: e.tensor_single_scalar(out=m.a, in_=r_, scalar=PI, op=ALU.is_gt), reads=[cs], writes=[m])
            c.op('dve', lambda e, r_=r_: e.scalar_tensor_tensor(out=r_, in0=m.a, scalar=-2 * PI, in1=r_, op0=ALU.mult, op1=ALU.add), reads=[m, cs], writes=[cs])
            c.op('dve', lambda e, r_=r_: e.tensor_single_scalar(out=m.a, in_=r_, scalar=-PI, op=ALU.is_lt), reads=[cs], writes=[m])
            c.op('dve', lambda e, r_=r_: e.scalar_tensor_tensor(out=r_, in0=m.a, scalar=2 * PI, in1=r_, op0=ALU.mult, op1=ALU.add), reads=[m, cs], writes=[cs])
        c.op('act', lambda e: e.activation(out=cs.a, in_=cs.a, func=AF.Sin), reads=[cs], writes=[cs])

    def load_w(dst_ap, dstbuf, src, c0, ncols, r0=0, nk=16):
        c.dma('sp', lambda e: e.dma_start(out=dst_ap, in_=src.a[r0:r0 + nk * 128, c0:c0 + ncols].rearrange("(k p) n -> p k n", p=128)),
              reads=[src], writes=[dstbuf])

    def proj_rot(wbuf, wc0, cs, dstT, rt):
        for mb in range(NMB):
            P = []
            for cc in range(2):
                ps = bank("proj")
                for kc in range(16):
                    c.op('pe', lambda e, ps=ps, kc=kc, cc=cc, mb=mb: e.matmul(ps.a[:, 0:MB], lhsT=wbuf.a[:, kc, wc0 + cc * 128:wc0 + (cc + 1) * 128],
                         rhs=hnT.a[:, kc, HALO + mb * MB:HALO + (mb + 1) * MB], start=(kc == 0), stop=(kc == 15)), reads=[wbuf, hnT], writes=[ps])
                P.append(ps)
            sl = slice(mb * MB, (mb + 1) * MB)
            sin_, cos_ = cs.a[:, 0, sl], cs.a[:, 1, sl]
            tt = lambda o, a, b, op, rd, wr: c.op('dve', lambda e: e.tensor_tensor(out=o, in0=a, in1=b, op=op), reads=rd, writes=wr)
            tt(rt[0].a, P[0].a[:, 0:MB], cos_, ALU.mult, [P[0], cs], [rt[0]])
            tt(rt[1].a, P[1].a[:, 0:MB], sin_, ALU.mult, [P[1], cs], [rt[1]])
            tt(dstT.a[:, 0, sl], rt[0].a, rt[1].a, ALU.subtract, [rt[0], rt[1]], [dstT])
            tt(rt[2].a, P[1].a[:, 0:MB], cos_, ALU.mult, [P[1], cs], [rt[2]])
            tt(rt[3].a, P[0].a[:, 0:MB], sin_, ALU.mult, [P[0], cs], [rt[3]])
            tt(dstT.a[:, 1, sl], rt[2].a, rt[3].a, ALU.add, [rt[2], rt[3]], [dstT])

    def k_tok(kT, k_s, kd_ap_fn):
        for t in range(NT):
            pt = bank("tr"); ptb = pt.a.bitcast(BF16)
            for cc in range(2):
                c.op('pe', lambda e, cc=cc, t=t, ptb=ptb: e.transpose(out=ptb[:, cc * 128:(cc + 1) * 128], in_=kT.a[:, cc, t * 128:(t + 1) * 128], identity=identb.a),
                     reads=[kT, identb], writes=[pt])
            c.op('act', lambda e, t=t, ptb=ptb: e.activation(out=k_s.a[:, t, :], in_=ptb[:, 0:256], func=AF.Copy, scale=kd_ap_fn(t)), reads=[pt, cst], writes=[k_s])

    def v_proj(wbuf, v_tok):
        for t in range(NT):
            ps = bank("proj")
            for kc in range(16):
                c.op('pe', lambda e, ps=ps, kc=kc, t=t: e.matmul(ps.a[:, 0:512], lhsT=hnT.a[:, kc, HALO + t * 128:HALO + (t + 1) * 128], rhs=wbuf.a[:, kc, :],
                     start=(kc == 0), stop=(kc == 15)), reads=[wbuf, hnT], writes=[ps])
            c.op('act', lambda e, ps=ps, t=t: e.activation(out=v_tok.a[:, t, :], in_=ps.a[:, 0:512], func=AF.Copy), reads=[ps], writes=[v_tok])

    def alloc_B():
        AR.reset()
        B = {}
        B["w0"] = AR.get("w0", [128, 16, 512]); B["w1"] = AR.get("w1", [128, 16, 512]); B["w2"] = AR.get("w2", [128, 16, 512])
        B["kT"] = AR.get("kT", [128, 2, BLK]); B["qT"] = AR.get("qT", [128, 2, BLK]); B["qTs"] = AR.get("qTs", [128, 2, BLK])
        B["k_s"] = AR.get("k_s", [128, NT, 256]); B["v_tok"] = AR.get("v_tok", [128, NT, 512]); B["gTs"] = AR.get("gTs", [128, 4, BLK])
        B["cs"] = AR.get("cs", [128, 2, BLK], F32)
        B["rt"] = [AR.get("rt%d" % i, [128, MB], F32) for i in range(4)]
        B["Sbf"] = AR.get("Sbf", [128, 2, 512])
        B["yn"] = AR.get("yn", [128, 512]); B["sT"] = AR.get("sT", [128, 128]); B["rT"] = AR.get("rT", [128, 4, 128])
        B["ln"] = AR.get("ln", [128, 12], F32)
        return B

    def alloc_P():
        P = {}
        P["wk"] = [AR.get("wk%d" % i, [128, 16, 256]) for i in range(2)]
        P["wv"] = [AR.get("wv%d" % i, [128, 16, 512]) for i in range(2)]
        P["kT"] = AR.get("kT", [128, 2, BLK]); P["k_s"] = AR.get("k_s", [128, NT, 256]); P["v_tok"] = AR.get("v_tok", [128, NT, 512])
        P["cs"] = AR.get("cs", [128, 2, BLK], F32)
        P["rt"] = [AR.get("rt%d" % i, [128, MB], F32) for i in range(4)]
        return P

    if "A" in stages:
        AR.reset()
        SA = alloc_A()
        P = alloc_P()
        RT = (AR.get("pi", [128, BLK], I32), AR.get("ang", [128, BLK], F32), AR.get("kf", [128, BLK], F32))
        for p in range(NPRE):
            stage_A(x_pre, p * BLK, SA)
            def ldkv(h):
                load_w(P["wk"][h % 2].a, P["wk"][h % 2], win_b, C_K + h * 256, 256)
                load_w(P["wv"][h % 2].a, P["wv"][h % 2], win_b, C_V + h * 512, 512)
            ldkv(0)
            rope_tables(pos_pre, p * BLK, P["cs"], RT)
            for h in range(HEADS):
                if h + 1 < HEADS:
                    ldkv(h + 1)
                if p >= 1:
                    trickle(2 if p >= NPRE - 2 else 1)
                if GAM[h] ** (BLK * (NPRE - 1 - p)) < PREFIX_SKIP:
                    continue
                wk_, wv_ = P["wk"][h % 2], P["wv"][h % 2]
                proj_rot(wk_, 0, P["cs"], P["kT"], P["rt"])
                v_proj(wv_, P["v_tok"])
                kd0 = lay["kdpre"][0]
                k_tok(P["kT"], P["k_s"], lambda t, h=h: cst.a[:, kd0 + h * NT + t:kd0 + h * NT + t + 1])
                co0 = lay["coef"][0]
                for cc in range(2):
                    pa = bank("acc")
                    for t in range(NT):
                        c.op('pe', lambda e, pa=pa, t=t, cc=cc: e.matmul(pa.a[:, 0:512], lhsT=P["k_s"].a[:, t, cc * 128:(cc + 1) * 128], rhs=P["v_tok"].a[:, t, :],
                             start=(t == 0), stop=(t == NT - 1)), reads=[P["k_s"], P["v_tok"]], writes=[pa])
                    c.op('dve', lambda e, pa=pa, cc=cc, h=h, p=p: e.scalar_tensor_tensor(out=Sst[h].a[:, cc, :], in0=pa.a[:, 0:512],
                         scalar=cst.a[:, co0 + p * 8 + h:co0 + p * 8 + h + 1], in1=Sst[h].a[:, cc, :], op0=ALU.mult, op1=ALU.add), reads=[pa, cst, Sst[h]], writes=[Sst[h]])
        c.barrier(skip_pool_dma=True)

    trickle(10 ** 9, upto=n_weight_pieces)
    for s in range(NSUB):
        tok0 = s * BLK
        stage_A(x_own, tok0)
        c.barrier(skip_pool_dma=(s == 0))
        B = alloc_B()
        rope_tables(pos_own, tok0, B["cs"])
        dm0 = lay["dmaskT"][0]; qd0 = lay["qdec"][0]; ko0 = lay["kdown"][0]
        def ld_qk(h):
            load_w(B["w0"].a[:, :, 0:256], B["w0"], win_b, C_Q + h * 256, 256)
            load_w(B["w0"].a[:, :, 256:512], B["w0"], win_b, C_K + h * 256, 256)
        def ld_v(h):
            load_w(B["w1"].a, B["w1"], win_b, C_V + h * 512, 512)
        def ld_g(h):
            load_w(B["w2"].a, B["w2"], win_b, C_G + h * 512, 512)
        for h in range(HEADS):
            if h == 0:
                ld_qk(0); ld_v(0); ld_g(0)
            if s == 0:
                trickle(4)
            proj_rot(B["w0"], 0, B["cs"], B["qT"], B["rt"])
            proj_rot(B["w0"], 256, B["cs"], B["kT"], B["rt"])
            if h + 1 < HEADS:
                ld_qk(h + 1)
            v_proj(B["w1"], B["v_tok"])
            if h + 1 < HEADS:
                ld_v(h + 1)
            for cc in range(2):
                c.op('dve', lambda e, cc=cc, h=h: e.tensor_tensor(out=B["qTs"].a[:, cc, :].rearrange("p (n i) -> p n i", i=128),
                     in0=B["qT"].a[:, cc, :].rearrange("p (n i) -> p n i", i=128),
                     in1=cst.a[:, qd0 + h * 128:qd0 + (h + 1) * 128].unsqueeze(1).to_broadcast([128, NT, 128]), op=ALU.mult),
                     reads=[B["qT"], cst], writes=[B["qTs"]])
            k_tok(B["kT"], B["k_s"], lambda t, h=h: cst.a[:, ko0 + h:ko0 + h + 1])
            for vc in range(4):
                for mb in range(NMB):
                    ps = bank("proj")
                    for kc in range(16):
                        c.op('pe', lambda e, ps=ps, kc=kc, vc=vc, mb=mb: e.matmul(ps.a[:, 0:MB], lhsT=B["w2"].a[:, kc, vc * 128:(vc + 1) * 128],
                             rhs=hnT.a[:, kc, HALO + mb * MB:HALO + (mb + 1) * MB], start=(kc == 0), stop=(kc == 15)), reads=[B["w2"], hnT], writes=[ps])
                    c.op('act', lambda e, ps=ps, vc=vc, mb=mb: e.activation(out=B["gTs"].a[:, vc, mb * MB:(mb + 1) * MB], in_=ps.a[:, 0:MB], func=AF.Silu),
                         reads=[ps], writes=[B["gTs"]])
            if h + 1 < HEADS:
                ld_g(h + 1)
            c.op('act', lambda e, h=h: e.activation(out=B["Sbf"].a, in_=Sst[h].a, func=AF.Copy), reads=[Sst[h]], writes=[B["Sbf"]])
            for n in range(NT):
                sl = slice(n * 128, (n + 1) * 128)
                psc = bank("tr")
                for cc in range(2):
                    c.op('pe', lambda e, cc=cc, sl=sl, psc=psc: e.matmul(psc.a[:, 0:128], lhsT=B["kT"].a[:, cc, sl], rhs=B["qT"].a[:, cc, sl],
                         start=(cc == 0), stop=(cc == 1)), reads=[B["kT"], B["qT"]], writes=[psc])
                c.op('dve', lambda e, psc=psc, h=h: e.tensor_tensor(out=B["sT"].a, in0=psc.a[:, 0:128], in1=cst.a[:, dm0 + h * 128:dm0 + (h + 1) * 128], op=ALU.mult),
                     reads=[psc, cst], writes=[B["sT"]])
                po = bank("proj")
                c.op('pe', lambda e, po=po, n=n: e.matmul(po.a[:, 0:512], lhsT=B["sT"].a, rhs=B["v_tok"].a[:, n, :], start=True, stop=False),
                     reads=[B["sT"], B["v_tok"]], writes=[po])
                for cc in range(2):
                    c.op('pe', lambda e, po=po, cc=cc, sl=sl: e.matmul(po.a[:, 0:512], lhsT=B["qTs"].a[:, cc, sl], rhs=B["Sbf"].a[:, cc, :], start=False, stop=(cc == 1)),
                         reads=[B["qTs"], B["Sbf"]], writes=[po])
                for cc in range(2):
                    pa = bank("acc")
                    c.op('pe', lambda e, pa=pa, cc=cc, n=n: e.matmul(pa.a[:, 0:512], lhsT=B["k_s"].a[:, n, cc * 128:(cc + 1) * 128], rhs=B["v_tok"].a[:, n, :], start=True, stop=True),
                         reads=[B["k_s"], B["v_tok"]], writes=[pa])
                    c.op('dve', lambda e, pa=pa, cc=cc, h=h: e.scalar_tensor_tensor(out=Sst[h].a[:, cc, :], in0=Sst[h].a[:, cc, :], scalar=float(GAM[h] ** 128),
                         in1=pa.a[:, 0:512], op0=ALU.mult, op1=ALU.add), reads=[pa, Sst[h]], writes=[Sst[h]])
                c.op('act', lambda e, h=h: e.activation(out=B["Sbf"].a, in_=Sst[h].a, func=AF.Copy), reads=[Sst[h]], writes=[B["Sbf"]])
                ln = B["ln"]
                c.op('dve', lambda e, po=po: e.bn_stats(out=ln.a[:, 0:6], in_=po.a[:, 0:512]), reads=[po], writes=[ln])
                c.op('dve', lambda e: e.bn_aggr(out=ln.a[:, 6:8], in_=ln.a[:, 0:6]), reads=[ln], writes=[ln])
                c.op('dve', lambda e: e.tensor_scalar(out=ln.a[:, 8:9], in0=ln.a[:, 7:8], scalar1=EPS, scalar2=None, op0=ALU.add), reads=[ln], writes=[ln])
                c.op('act', lambda e: e.activation(out=ln.a[:, 9:10], in_=ln.a[:, 8:9], func=AF.Sqrt), reads=[ln], writes=[ln])
                c.op('dve', lambda e: e.reciprocal(out=ln.a[:, 10:11], in_=ln.a[:, 9:10]), reads=[ln], writes=[ln])
                c.op('dve', lambda e, po=po: e.tensor_scalar(out=B["yn"].a, in0=po.a[:, 0:512], scalar1=ln.a[:, 6:7], scalar2=ln.a[:, 10:11], op0=ALU.subtract, op1=ALU.mult),
                     reads=[po, ln], writes=[B["yn"]])
                pt = bank("tr"); ptb = pt.a.bitcast(BF16)
                for vc in range(4):
                    c.op('pe', lambda e, vc=vc, ptb=ptb: e.transpose(out=ptb[:, vc * 128:(vc + 1) * 128], in_=B["yn"].a[:, vc * 128:(vc + 1) * 128], identity=identb.a),
                         reads=[B["yn"], identb], writes=[pt])
                for vc in range(4):
                    c.op('dve', lambda e, vc=vc, ptb=ptb, h=h, sl=sl: e.scalar_tensor_tensor(out=B["rT"].a[:, vc, :], in0=ptb[:, vc * 128:(vc + 1) * 128],
                         scalar=vecT2.a[:, 16 + h * 4 + vc:17 + h * 4 + vc], in1=B["gTs"].a[:, vc, sl], op0=ALU.mult, op1=ALU.mult), reads=[pt, vecT2, B["gTs"]], writes=[B["rT"]])
                c.dma('sp', lambda e, h=h, n=n: e.dma_start(out=ret_scr.a[h * 512:(h + 1) * 512, tok0 + n * 128:tok0 + (n + 1) * 128].rearrange("(v p) t -> p v t", p=128),
                      in_=B["rT"].a), reads=[B["rT"]], writes=[ret_scr])
        c.barrier(skip_pool_dma=(s == 0))
        if "C" not in stages:
            continue
        for tb in range(NMB):
            AR.reset()
            W = [AR.get("cw%d" % i, [128, 16, 512]) for i in range(3)]
            seq = []
            for g_ in range(4):
                seq.append((win_b, C_U + g_ * 512, 0))
            for nb_ in range(4):
                seq += [(wbp_b, nb_ * 512, 0), (win_b, C_AP + nb_ * 512, 0), (wbr_b, nb_ * 512, 0), (wbr_b, nb_ * 512, 2048), (win_b, C_AR + nb_ * 512, 0)]
            for db_ in range(4):
                seq.append((wout_b, db_ * 512, 0))
            wst = {"ptr": 0, "issued": 0}
            def nextw():
                i = wst["ptr"]
                while wst["issued"] <= min(i + 1, len(seq) - 1):
                    j = wst["issued"]
                    src_, c0_, r0_ = seq[j]
                    load_w(W[j % 3].a, W[j % 3], src_, c0_, 512, r0=r0_)
                    wst["issued"] += 1
                wst["ptr"] += 1
                return W[i % 3]
            U = AR.get("U", [128, HALO + MB], F32); ta = AR.get("ta", [128, HALO + MB], F32); tb_ = AR.get("tb", [128, HALO + MB], F32)
            pooledT = AR.get("pooledT", [128, 4, MB]); pw = AR.get("pw", [128, 4, 512])
            pool_outT = AR.get("pool_outT", [128, 16, MB]); mergedT = AR.get("mergedT", [128, 16, MB])
            retT = AR.get("retT", [128, 16, MB])
            ga = AR.get("ga", [128, MB]); gr = AR.get("gr", [128, MB]); t1 = AR.get("t1", [128, MB], F32); t2 = AR.get("t2", [128, MB], F32)
            xp = [AR.get("xp%d" % i, [128, 512], F32) for i in range(2)]
            hc0 = tb * MB
            first = (s == 0 and tb == 0)
            pc0 = lay["pcorr"][0]; hm_ap = CS("hm")
            for g in range(4):
                w_ = POOLW[g]
                wu = nextw()
                load_w(pw.a, pw, poolw_b, 0, 512, r0=g * 512, nk=4)
                for cc in range(4):
                    ph = bank("acc"); pm_ = bank("proj")
                    for kc in range(16):
                        c.op('pe', lambda e, kc=kc, cc=cc, ph=ph: e.matmul(ph.a[:, 0:HALO], lhsT=wu.a[:, kc, cc * 128:(cc + 1) * 128], rhs=hnT.a[:, kc, hc0:hc0 + HALO],
                             start=(kc == 0), stop=(kc == 15)), reads=[wu, hnT], writes=[ph])
                    for kc in range(16):
                        c.op('pe', lambda e, kc=kc, cc=cc, pm_=pm_: e.matmul(pm_.a[:, 0:MB], lhsT=wu.a[:, kc, cc * 128:(cc + 1) * 128], rhs=hnT.a[:, kc, hc0 + HALO:hc0 + HALO + MB],
                             start=(kc == 0), stop=(kc == 15)), reads=[wu, hnT], writes=[pm_])
                    if first:
                        c.op('dve', lambda e, ph=ph: e.tensor_scalar(out=U.a[:, 0:HALO], in0=ph.a[:, 0:HALO], scalar1=hm_ap, scalar2=None, op0=ALU.mult), reads=[ph, cst], writes=[U])
                    else:
                        c.op('act', lambda e, ph=ph: e.activation(out=U.a[:, 0:HALO], in_=ph.a[:, 0:HALO], func=AF.Copy), reads=[ph], writes=[U])
                    c.op('act', lambda e, pm_=pm_: e.activation(out=U.a[:, HALO:], in_=pm_.a[:, 0:MB], func=AF.Copy), reads=[pm_], writes=[U])
                    L = HALO + MB
                    src, dst, sh = U, ta, 1
                    while sh < w_:
                        c.op('dve', lambda e, src=src, dst=dst, sh=sh: e.tensor_tensor(out=dst.a[:, 2 * sh - 1:L], in0=src.a[:, 2 * sh - 1:L], in1=src.a[:, sh - 1:L - sh], op=ALU.add),
                             reads=[src], writes=[dst])
                        src = dst
                        dst = tb_ if dst is ta else ta
                        sh *= 2
                    if first:
                        c.op('dve', lambda e, src=src, g=g: e.tensor_tensor(out=src.a[:, HALO:2 * HALO], in0=src.a[:, HALO:2 * HALO], in1=cst.a[:, pc0 + g * 16:pc0 + (g + 1) * 16], op=ALU.mult),
                             reads=[src, cst], writes=[src])
                    c.op('dve', lambda e, src=src, cc=cc: e.scalar_tensor_tensor(out=pooledT.a[:, cc, :], in0=src.a[:, HALO:], scalar=1.0 / w_, in1=U.a[:, HALO:], op0=ALU.mult, op1=ALU.subtract),
                         reads=[src, U], writes=[pooledT])
                for dc in range(4):
                    pq = bank("proj")
                    for k4 in range(4):
                        c.op('pe', lambda e, pq=pq, k4=k4, dc=dc: e.matmul(pq.a[:, 0:MB], lhsT=pw.a[:, k4, dc * 128:(dc + 1) * 128], rhs=pooledT.a[:, k4, :], start=(k4 == 0), stop=(k4 == 3)),
                             reads=[pw, pooledT], writes=[pq])
                    c.op('act', lambda e, pq=pq, dc=dc, g=g: e.activation(out=pool_outT.a[:, g * 4 + dc, :], in_=pq.a[:, 0:MB], func=AF.Copy, scale=vecT2.a[:, g * 4 + dc:g * 4 + dc + 1]),
                         reads=[pq, vecT2], writes=[pool_outT])
            for nb in range(4):
                wp = nextw()
                wa = nextw()
                T1 = []
                for nn in range(4):
                    pp = bank("proj")
                    for kc in range(16):
                        c.op('pe', lambda e, pp=pp, kc=kc, nn=nn: e.matmul(pp.a[:, 0:MB], lhsT=wp.a[:, kc, nn * 128:(nn + 1) * 128], rhs=pool_outT.a[:, kc, :], start=(kc == 0), stop=(kc == 15)),
                             reads=[wp, pool_outT], writes=[pp])
                    pa = bank("acc")
                    for kc in range(16):
                        c.op('pe', lambda e, pa=pa, kc=kc, nn=nn: e.matmul(pa.a[:, 0:MB], lhsT=wa.a[:, kc, nn * 128:(nn + 1) * 128], rhs=hnT.a[:, kc, hc0 + HALO:hc0 + HALO + MB], start=(kc == 0), stop=(kc == 15)),
                             reads=[wa, hnT], writes=[pa])
                    c.op('act', lambda e, pa=pa: e.activation(out=ga.a, in_=pa.a[:, 0:MB], func=AF.Sigmoid), reads=[pa], writes=[ga])
                    c.op('dve', lambda e, pp=pp, nn=nn, nb=nb: e.tensor_tensor(out=mergedT.a[:, nb * 4 + nn, :], in0=pp.a[:, 0:MB], in1=ga.a, op=ALU.mult), reads=[pp, ga], writes=[mergedT])
                PR = [bank("proj") for _ in range(4)]
                for kh in range(2):
                    wr_ = nextw()
                    c.dma('sp', lambda e, kh=kh: e.dma_start(out=retT.a, in_=ret_scr.a[kh * 2048:(kh + 1) * 2048, tok0 + tb * MB:tok0 + (tb + 1) * MB].rearrange("(k p) t -> p k t", p=128)),
                          reads=[ret_scr], writes=[retT])
                    for nn in range(4):
                        for kc in range(16):
                            c.op('pe', lambda e, nn=nn, kc=kc, kh=kh, wr_=wr_: e.matmul(PR[nn].a[:, 0:MB], lhsT=wr_.a[:, kc, nn * 128:(nn + 1) * 128], rhs=retT.a[:, kc, :],
                                 start=(kh == 0 and kc == 0), stop=(kh == 1 and kc == 15)), reads=[wr_, retT], writes=[PR[nn]])
                wa2 = nextw()
                for nn in range(4):
                    pa = bank("acc")
                    for kc in range(16):
                        c.op('pe', lambda e, pa=pa, kc=kc, nn=nn: e.matmul(pa.a[:, 0:MB], lhsT=wa2.a[:, kc, nn * 128:(nn + 1) * 128], rhs=hnT.a[:, kc, hc0 + HALO:hc0 + HALO + MB], start=(kc == 0), stop=(kc == 15)),
                             reads=[wa2, hnT], writes=[pa])
                    c.op('act', lambda e, pa=pa: e.activation(out=gr.a, in_=pa.a[:, 0:MB], func=AF.Sigmoid), reads=[pa], writes=[gr])
                    c.op('dve', lambda e, nn=nn: e.tensor_tensor(out=t2.a, in0=PR[nn].a[:, 0:MB], in1=gr.a, op=ALU.mult), reads=[PR[nn], gr], writes=[t2])
                    c.op('dve', lambda e, nn=nn, nb=nb: e.tensor_tensor(out=mergedT.a[:, nb * 4 + nn, :], in0=mergedT.a[:, nb * 4 + nn, :], in1=t2.a, op=ALU.add), reads=[mergedT, t2], writes=[mergedT])
            for db in range(4):
                wo = nextw()
                for tt in range(MB // 128):
                    r0 = tok0 + tb * MB + tt * 128
                    py = bank("proj")
                    for kc in range(16):
                        c.op('pe', lambda e, py=py, kc=kc, tt=tt: e.matmul(py.a[:, 0:512], lhsT=mergedT.a[:, kc, tt * 128:(tt + 1) * 128], rhs=wo.a[:, kc, :], start=(kc == 0), stop=(kc == 15)),
                             reads=[mergedT, wo], writes=[py])
                    xq = xp[(db * 4 + tt) % 2]
                    c.dma('sp', lambda e, xq=xq, r0=r0, db=db: e.dma_start(out=xq.a, in_=x_own.a[r0:r0 + 128, db * 512:(db + 1) * 512]), reads=[x_own], writes=[xq])
                    c.op('dve', lambda e, py=py, db=db: e.tensor_tensor(out=t1.a[:, 0:512] if MB >= 512 else t1.a, in0=py.a[:, 0:512], in1=g1bc.a[:, db * 512:(db + 1) * 512], op=ALU.mult), reads=[py, g1bc], writes=[t1])
                    c.op('dve', lambda e, xq=xq: e.tensor_tensor(out=xq.a, in0=xq.a, in1=t1.a[:, 0:512], op=ALU.add), reads=[xq, t1], writes=[xq])
                    c.dma('sp', lambda e, xq=xq, r0=r0, db=db: e.dma_start(out=x1_scr.a[r0:r0 + 128, db * 512:(db + 1) * 512], in_=xq.a), reads=[xq], writes=[x1_scr])
            c.barrier(skip_pool_dma=(s == 0))
        if "D" not in stages:
            continue
        trickle(10 ** 9)
        AR.reset()
        keysT = AR.get("keysT", [128, 16, 128], F32)
        fnT = AR.get("fnT", [128, 16, MB], F32)
        mark = AR.off
        kraw = AR.get("kraw", [128, 16, 128], F32)
        c.dma('sp', lambda e: e.dma_start(out=kraw.a, in_=keys.a.rearrange("(g n) c -> n g c", n=128)), reads=[keys], writes=[kraw])
        for g4 in range(4):
            pt = bank("tr")
            for j in range(4):
                c.op('pe', lambda e, pt=pt, j=j, g4=g4: e.transpose(out=pt.a[:, j * 128:(j + 1) * 128], in_=kraw.a[:, g4 * 4 + j, :], identity=ident), reads=[kraw, cst], writes=[pt])
            c.op('dve', lambda e, pt=pt, g4=g4: e.tensor_copy(out=keysT.a[:, g4 * 4:(g4 + 1) * 4, :], in_=pt.a[:, 0:512].rearrange("p (a b) -> p a b", a=4)), reads=[pt], writes=[keysT])
        c.barrier()
        for mbk in range(NMB):
            AR.off = mark
            xt = AR.get("xtD", [128, D], F32); xs2 = AR.get("xs2", [128, D], F32); st = AR.get("stD", [128, 4], F32)
            fnt = AR.get("fnt", [128, D], BF16)
            for tl in range(MB // 128):
                t = mbk * (MB // 128) + tl
                r0 = tok0 + t * 128
                c.dma('sp', lambda e, r0=r0: e.dma_start(out=xt.a, in_=x1_scr.a[r0:r0 + 128, :]), reads=[x1_scr], writes=[xt])
                c.op('dve', lambda e: e.memset(st.a[:, 0:1], 0.0), writes=[st])
                c.op('act', lambda e: e.activation(out=xs2.a, in_=xt.a, func=AF.Square, accum_out=st.a[:, 0:1]), reads=[xt, st], writes=[xs2, st])
                c.op('dve', lambda e: e.tensor_scalar(out=st.a[:, 1:2], in0=st.a[:, 0:1], scalar1=1.0 / D, scalar2=EPS, op0=ALU.mult, op1=ALU.add), reads=[st], writes=[st])
                c.op('act', lambda e: e.activation(out=st.a[:, 2:3], in_=st.a[:, 1:2], func=AF.Sqrt), reads=[st], writes=[st])
                c.op('dve', lambda e: e.reciprocal(out=st.a[:, 3:4], in_=st.a[:, 2:3]), reads=[st], writes=[st])
                c.op('dve', lambda e: e.tensor_scalar(out=xs2.a, in0=xt.a, scalar1=st.a[:, 3:4], scalar2=None, op0=ALU.mult), reads=[xt, st], writes=[xs2])
                for q4 in range(4):
                    pt = bank("tr")
                    for j in range(4):
                        kc = q4 * 4 + j
                        c.op('pe', lambda e, pt=pt, j=j, kc=kc: e.transpose(out=pt.a[:, j * 128:(j + 1) * 128], in_=xs2.a[:, kc * 128:(kc + 1) * 128], identity=ident), reads=[xs2, cst], writes=[pt])
                    for j in range(4):
                        kc = q4 * 4 + j
                        c.op('act', lambda e, pt=pt, j=j, kc=kc, tl=tl: e.activation(out=fnT.a[:, kc, tl * 128:(tl + 1) * 128], in_=pt.a[:, j * 128:(j + 1) * 128], func=AF.Identity,
                             scale=AB.a[:, 2, kc:kc + 1], bias=AB.a[:, 3, kc:kc + 1]), reads=[pt, AB], writes=[fnT])
                for q4 in range(4):
                    pt = bank("tr")
                    for j in range(4):
                        kc = q4 * 4 + j
                        c.op('pe', lambda e, pt=pt, j=j, kc=kc, tl=tl: e.transpose(out=pt.a[:, j * 128:(j + 1) * 128], in_=fnT.a[:, kc, tl * 128:(tl + 1) * 128], identity=ident), reads=[fnT, cst], writes=[pt])
                    c.op('act', lambda e, pt=pt, q4=q4: e.activation(out=fnt.a[:, q4 * 512:(q4 + 1) * 512], in_=pt.a[:, 0:512], func=AF.Copy), reads=[pt], writes=[fnt])
                c.dma('sp', lambda e, t=t: e.dma_start(out=fn_scr.a[t * 128:(t + 1) * 128, :], in_=fnt.a), reads=[fnt], writes=[fn_scr])
            c.barrier()
            AR.off = mark
            wqb = [AR.get("wq%d" % i, [128, 16, 512], F32) for i in range(2)]
            qTc = [AR.get("qTc%d" % i, [128, MB], F32) for i in range(2)]
            s_hp = [AR.get("s_hp%d" % i, [128, MB // 128, 128], F32) for i in range(2)]
            ntl = MB // 128
            for wbk in range(4):
                wq_ = wqb[wbk % 2]
                c.dma('sp', lambda e, wq_=wq_, wbk=wbk: e.dma_start(out=wq_.a, in_=wq.a[:, wbk * 512:(wbk + 1) * 512].rearrange("(k p) n -> p k n", p=128)), reads=[wq], writes=[wq_])
                for j in range(4):
                    hp = wbk * 4 + j
                    qc = qTc[hp % 2]; sh_ = s_hp[hp % 2]
                    ps = bank("proj")
                    for kc in range(16):
                        c.op('pe', lambda e, ps=ps, kc=kc, j=j, wq_=wq_: e.matmul(ps.a[:, 0:MB], lhsT=wq_.a[:, kc, j * 128:(j + 1) * 128], rhs=fnT.a[:, kc, :],
                             start=(kc == 0), stop=(kc == 15)), reads=[wq_, fnT], writes=[ps])
                    c.op('act', lambda e, ps=ps, qc=qc: e.activation(out=qc.a, in_=ps.a[:, 0:MB], func=AF.Copy), reads=[ps], writes=[qc])
                    pss = bank("acc")
                    for tl in range(ntl):
                        c.op('pe', lambda e, pss=pss, tl=tl, qc=qc, hp=hp: e.matmul(pss.a[:, tl * 128:(tl + 1) * 128], lhsT=qc.a[:, tl * 128:(tl + 1) * 128], rhs=keysT.a[:, hp, :],
                             start=True, stop=True), reads=[qc, keysT], writes=[pss])
                    c.op('dve', lambda e, pss=pss, sh_=sh_: e.tensor_copy(out=sh_.a, in_=pss.a[:, 0:ntl * 128].rearrange("p (a b) -> p a b", a=ntl)), reads=[pss], writes=[sh_])
                    c.dma('sp', lambda e, sh_=sh_, hp=hp, mbk=mbk: e.dma_start(out=s_scr.a[mbk * MB:(mbk + 1) * MB, hp * 128:(hp + 1) * 128].rearrange("(t p) n -> p t n", p=128), in_=sh_.a),
                          reads=[sh_], writes=[s_scr])
            c.barrier()
        AR.reset()
        sT_ = AR.get("sD", [128, 16, 128], F32); sW = AR.get("sW", [128, 16, 128], F32)
        x1 = AR.get("x1", [128, D], F32)
        GK = 4
        dg = [AR.get("dg%d" % i, [128, GK, 128], BF16) for i in range(2)]
        tmpD = AR.get("tmpD", [128, 512], F32)
        g2b = AR.get("g2b", [128, D], F32); fgb = AR.get("fgb", [128, D], F32)
        gat = [AR.get("gat%d" % i, [128, 2 * D], BF16) for i in range(5)]
        if BLK >= 1024:
            gat += [Buf("gath%d" % i, hn_raw.ap()[:, i * 4096:(i + 1) * 4096]) for i in range(4)]
        NG = len(gat)
        tops = AR.get("tops", [128, 16, 16], F32); topi = AR.get("topi", [128, 16, 16], U32); topf = AR.get("topf", [128, 16, 16], F32)
        cand = AR.get("cand", [128, 8, 256], F32); candw = AR.get("candw", [128, 8, 256], F32)
        bs = AR.get("bs", [128, 8, 16], F32); bp = AR.get("bp", [128, 8, 16], U32); r01 = AR.get("r01", [128, 2, 8, 16], U32); r01f = AR.get("r01f", [128, 2, 8, 16], F32)
        ij = AR.get("ij", [128, 2, 8, 16], F32)
        oh = candw; oh4 = candw.a.rearrange("p h (a b) -> p h a b", a=16)
        junk = sW; junk2 = sW.a.rearrange("p g n -> p (g n)")
        ef = AR.get("ef", [128, 128], F32); eidx = AR.get("eidx", [128, 128], U32)
        gts = AR.get("gts", [128, 8, 16], F32); gsum = AR.get("gsum", [128, 8], F32)
        pre = AR.get("pre", [128, 128], F32); ge = AR.get("ge", [128, 4, 128], F32); wgt = AR.get("wgt", [128, 128], F32)
        stD = AR.get("stD2", [128, 4], F32)
        fnb = AR.get("fnb", [128, D], BF16)
        junkb = sW.a.rearrange("p g n -> p (g n)").bitcast(BF16)[:, 0:D]
        iota = CS("iota")
        c.dma('sp', lambda e: e.dma_start(out=g2b.a, in_=g2row.a[0:1, :].partition_broadcast(128)), reads=[g2row], writes=[g2b])
        c.dma('sp', lambda e: e.dma_start(out=fgb.a, in_=fgain.a[0:1, :].partition_broadcast(128)), reads=[fgain], writes=[fgb])
        def top16(src, work, nvals, vout, iout):
            c.op('dve', lambda e: e.max(out=vout[1][:, 0:8], in_=src[1]), reads=[src[0]], writes=[vout[0]])
            c.op('dve', lambda e: e.max_index(out=iout[1][:, 0:8], in_max=vout[1][:, 0:8], in_values=src[1]), reads=[src[0], vout[0]], writes=[iout[0]])
            c.op('dve', lambda e: e.match_replace(out=work[1], in_to_replace=vout[1][:, 0:8], in_values=src[1], imm_value=-1e30), reads=[src[0], vout[0]], writes=[work[0]])
            c.op('dve', lambda e: e.max(out=vout[1][:, 8:16], in_=work[1]), reads=[work[0]], writes=[vout[0]])
            c.op('dve', lambda e: e.max_index(out=iout[1][:, 8:16], in_max=vout[1][:, 8:16], in_values=work[1]), reads=[work[0], vout[0]], writes=[iout[0]])
        for t in range(NT):
            r0 = tok0 + t * 128
            c.dma('sp', lambda e, t=t: e.dma_start(out=sT_.a, in_=s_scr.a[t * 128:(t + 1) * 128, :].rearrange("p (g n) -> p g n", g=16)), reads=[s_scr], writes=[sT_])
            c.dma('sp', lambda e, r0=r0: e.dma_start(out=x1.a, in_=x1_scr.a[r0:r0 + 128, :]), reads=[x1_scr], writes=[x1])
            c.dma('sp', lambda e, t=t: e.dma_start(out=fnb.a, in_=fn_scr.a[t * 128:(t + 1) * 128, :]), reads=[fn_scr], writes=[fnb])
            for g in range(16):
                top16((sT_, sT_.a[:, g, :]), (sW, sW.a[:, g, :]), 128, (tops, tops.a[:, g, :]), (topi, topi.a[:, g, :]))
            c.op('dve', lambda e: e.tensor_copy(out=topf.a, in_=topi.a), reads=[topi], writes=[topf])
            t4 = tops.a.rearrange("p (h two) r -> p h two r", two=2)
            c.op('dve', lambda e: e.tensor_tensor(out=cand.a.rearrange("p h (a b) -> p h a b", a=16), in0=t4[:, :, 0, :].unsqueeze(3).to_broadcast([128, 8, 16, 16]),
                 in1=t4[:, :, 1, :].unsqueeze(2).to_broadcast([128, 8, 16, 16]), op=ALU.add), reads=[tops], writes=[cand])
            for h in range(8):
                top16((cand, cand.a[:, h, :]), (candw, candw.a[:, h, :]), 256, (bs, bs.a[:, h, :]), (bp, bp.a[:, h, :]))
            c.op('dve', lambda e: e.tensor_single_scalar(out=r01.a[:, 0], in_=bp.a, scalar=4, op=ALU.logical_shift_right), reads=[bp], writes=[r01])
            c.op('dve', lambda e: e.tensor_single_scalar(out=r01.a[:, 1], in_=bp.a, scalar=15, op=ALU.bitwise_and), reads=[bp], writes=[r01])
            c.op('dve', lambda e: e.tensor_copy(out=r01f.a, in_=r01.a), reads=[r01], writes=[r01f])
            f4 = topf.a.rearrange("p (h two) r -> p h two r", two=2)
            for side in range(2):
                c.op('dve', lambda e, side=side: e.tensor_tensor(out=oh4, in0=r01f.a[:, side].unsqueeze(3).to_broadcast([128, 8, 16, 16]),
                     in1=iota.unsqueeze(1).unsqueeze(1).to_broadcast([128, 8, 16, 16]), op=ALU.is_equal), reads=[r01f, cst], writes=[oh])
                c.op('dve', lambda e, side=side: e.tensor_tensor(out=oh4, in0=oh4, in1=f4[:, :, side, :].unsqueeze(2).to_broadcast([128, 8, 16, 16]), op=ALU.mult), reads=[oh, topf], writes=[oh])
                c.op('dve', lambda e, side=side: e.tensor_reduce(out=ij.a[:, side], in_=oh4, axis=AX.X, op=ALU.add), reads=[oh], writes=[ij])
            c.op('dve', lambda e: e.scalar_tensor_tensor(out=ef.a.rearrange("p (h r) -> p h r", h=8), in0=ij.a[:, 0], scalar=128.0, in1=ij.a[:, 1], op0=ALU.mult, op1=ALU.add), reads=[ij], writes=[ef])
            c.op('dve', lambda e: e.tensor_copy(out=eidx.a, in_=ef.a), reads=[ef], writes=[eidx])
            c.op('dve', lambda e: e.tensor_tensor(out=gts.a, in0=bs.a, in1=bs.a[:, :, 0:1].to_broadcast([128, 8, 16]), op=ALU.subtract), reads=[bs], writes=[gts])
            c.op('act', lambda e: e.activation(out=gts.a, in_=gts.a, func=AF.Exp), reads=[gts], writes=[gts])
            c.op('dve', lambda e: e.tensor_reduce(out=gsum.a, in_=gts.a, axis=AX.X, op=ALU.add), reads=[gts], writes=[gsum])
            c.op('dve', lambda e: e.reciprocal(out=gsum.a, in_=gsum.a), reads=[gsum], writes=[gsum])
            c.op('dve', lambda e: e.tensor_tensor(out=gts.a, in0=gts.a, in1=gsum.a.unsqueeze(2).to_broadcast([128, 8, 16]), op=ALU.mult), reads=[gts, gsum], writes=[gts])
            PY = [pb[i] for i in range(4)]
            c.op('dve', lambda e: e.memset(pre.a, 0.0), writes=[pre])
            for grp in range(128 // GK):
                k0 = grp * GK
                sl = slice(k0, k0 + GK)
                for j in range(GK):
                    k = k0 + j
                    gb = gat[k % NG]
                    c.dma('pool', lambda e, gb=gb, k=k: e.indirect_dma_start(out=gb.a, out_offset=None, in_=uv_bf.a, in_offset=bass.IndirectOffsetOnAxis(ap=eidx.a[:, k:k + 1], axis=0)),
                          reads=[eidx, uv_bf], writes=[gb])
                    c.op('dve', lambda e, gb=gb, k=k: e.scalar_tensor_tensor(out=junkb, in0=gb.a[:, 0:D], scalar=1.0, in1=fnb.a, op0=ALU.mult, op1=ALU.mult, accum_out=pre.a[:, k:k + 1]),
                         reads=[gb, fnb, pre], writes=[junk, pre])
                c.op('dve', lambda e, sl=sl: e.tensor_tensor(out=ge.a[:, 0, sl], in0=pre.a[:, sl], in1=pre.a[:, sl], op=ALU.mult), reads=[pre], writes=[ge])
                c.op('dve', lambda e, sl=sl: e.tensor_scalar(out=ge.a[:, 1, sl], in0=ge.a[:, 0, sl], scalar1=0.044715, scalar2=1.0, op0=ALU.mult, op1=ALU.add), reads=[ge], writes=[ge])
                c.op('dve', lambda e, sl=sl: e.tensor_tensor(out=ge.a[:, 2, sl], in0=ge.a[:, 1, sl], in1=pre.a[:, sl], op=ALU.mult), reads=[ge, pre], writes=[ge])
                c.op('act', lambda e, sl=sl: e.activation(out=ge.a[:, 3, sl], in_=ge.a[:, 2, sl], func=AF.Sigmoid, scale=1.5957691216057308), reads=[ge], writes=[ge])
                c.op('dve', lambda e, sl=sl: e.tensor_tensor(out=wgt.a[:, sl], in0=ge.a[:, 3, sl], in1=pre.a[:, sl], op=ALU.mult), reads=[ge, pre], writes=[wgt])
                c.op('dve', lambda e, sl=sl: e.tensor_tensor(out=wgt.a[:, sl], in0=wgt.a[:, sl], in1=gts.a.rearrange("p h r -> p (h r)")[:, sl], op=ALU.mult), reads=[wgt, gts], writes=[wgt])
                dgk = dg[grp % 2]
                for j in range(GK):
                    c.op('act', lambda e, dgk=dgk, j=j, k0=k0: e.activation(out=dgk.a[:, j, :], in_=identb.a, func=AF.Copy, scale=wgt.a[:, k0 + j:k0 + j + 1]),
                         reads=[identb, wgt], writes=[dgk])
                for j in range(GK):
                    k = k0 + j
                    gb = gat[k % NG]
                    for nb in range(4):
                        c.op('pe', lambda e, gb=gb, k=k, nb=nb, dgk=dgk, j=j: e.matmul(PY[nb].a[:, 0:512], lhsT=dgk.a[:, j, :], rhs=gb.a[:, D + nb * 512:D + (nb + 1) * 512],
                             start=(k == 0), stop=(k == 127)), reads=[dgk, gb], writes=[PY[nb]])
            for nb in range(4):
                c.op('dve', lambda e, nb=nb: e.tensor_tensor(out=tmpD.a, in0=PY[nb].a[:, 0:512], in1=g2b.a[:, nb * 512:(nb + 1) * 512], op=ALU.mult), reads=[PY[nb], g2b], writes=[tmpD])
                c.op('dve', lambda e, nb=nb: e.tensor_tensor(out=x1.a[:, nb * 512:(nb + 1) * 512], in0=x1.a[:, nb * 512:(nb + 1) * 512], in1=tmpD.a, op=ALU.add), reads=[x1, tmpD], writes=[x1])
            c.op('dve', lambda e: e.memset(stD.a[:, 0:1], 0.0), writes=[stD])
            c.op('act', lambda e: e.activation(out=junk2, in_=x1.a, func=AF.Square, accum_out=stD.a[:, 0:1]), reads=[x1, stD], writes=[junk, stD])
            c.op('dve', lambda e: e.tensor_scalar(out=stD.a[:, 1:2], in0=stD.a[:, 0:1], scalar1=1.0 / D, scalar2=EPS, op0=ALU.mult, op1=ALU.add), reads=[stD], writes=[stD])
            c.op('act', lambda e: e.activation(out=stD.a[:, 2:3], in_=stD.a[:, 1:2], func=AF.Sqrt), reads=[stD], writes=[stD])
            c.op('dve', lambda e: e.reciprocal(out=stD.a[:, 3:4], in_=stD.a[:, 2:3]), reads=[stD], writes=[stD])
            c.op('dve', lambda e: e.scalar_tensor_tensor(out=x1.a, in0=x1.a, scalar=stD.a[:, 3:4], in1=fgb.a, op0=ALU.mult, op1=ALU.mult), reads=[x1, stD, fgb], writes=[x1])
            c.dma('sp', lambda e, r0=r0: e.dma_start(out=out.a[r0:r0 + 128, :], in_=x1.a), reads=[x1], writes=[out])
        c.barrier()
    c.finish()
    return nc, c, lay, NCONST


def _consts(core, BLK, NT, NPRE, lay, NCONST, TPC):
    cs = np.zeros((128, NCONST), np.float32)
    def put(nm, arr):
        o, w = lay[nm]
        cs[:, o:o + w] = arr
    put("ident", np.eye(128, dtype=np.float32))
    g = np.array(GAM, np.float64)
    i = np.arange(128)
    diff = i[None, :] - i[:, None]
    dm = np.zeros((128, 8, 128))
    for h in range(8):
        dm[:, h, :] = np.where(diff >= 0, g[h] ** np.maximum(diff, 0), 0.0) / 16.0
    put("dmaskT", dm.reshape(128, 1024))
    qd = np.stack([g[h] ** (i + 1.0) for h in range(8)], 0)
    put("qdec", np.broadcast_to(qd.reshape(1, 1024), (128, 1024)))
    put("kdown", np.stack([g[h] ** (127.0 - i) / 16.0 for h in range(8)], 1))
    kp = np.zeros((128, 8, NT))
    for h in range(8):
        for n in range(NT):
            kp[:, h, n] = g[h] ** (BLK - 1.0 - (128 * n + i)) / 16.0
    put("kdpre", kp.reshape(128, 8 * NT))
    own_start = core * TPC
    co = np.zeros((NPRE, 8))
    for p in range(NPRE):
        if own_start - (NPRE - p) * BLK >= 0:
            co[p] = g ** (BLK * (NPRE - 1.0 - p))
    put("coef", np.broadcast_to(co.reshape(1, NPRE * 8), (128, NPRE * 8)))
    invf = (np.float32(10000.0) ** (-(np.arange(0, 256, 2, dtype=np.float32)) / np.float32(256.0))).astype(np.float32)
    put("invf", invf.reshape(128, 1))
    put("hm", np.full((128, 1), 0.0 if core == 0 else 1.0))
    pc = np.ones((4, 16))
    if core == 0:
        for gi, w in enumerate(POOLW):
            pc[gi] = w / np.minimum(np.arange(16) + 1.0, w)
    put("pcorr", np.broadcast_to(pc.reshape(1, 64), (128, 64)))
    put("iota", np.broadcast_to(np.arange(16, dtype=np.float32).reshape(1, 16), (128, 16)))
    return cs


_CACHE = {}


def _run(inputs, SEQ, BLK, stages="ABCD", trace=False):
    f = lambda k: np.ascontiguousarray(np.asarray(inputs[k]))
    TPC = SEQ // NCORES
    NT = BLK // 128
    NPRE = 7 * (TPC // BLK)
    key = (SEQ, BLK, stages)
    if key not in _CACHE:
        _CACHE[key] = build_nc(SEQ, BLK, stages)
    nc, ctx, lay, NCONST = _CACHE[key]
    x = f("x")[0]
    pos = f("positions")[0].astype(np.int32)
    pvec1 = np.concatenate([f("b_ada")[0].reshape(96, 128), f("norm_mix_gain")[0].reshape(16, 128), f("norm_ffn_gain")[0].reshape(16, 128)], 0).astype(np.float32)
    pvec2 = np.concatenate([f("pool_scale")[0].reshape(16, 128), f("ret_norm_gain")[0].reshape(32, 128), f("c")[0].reshape(16, 128), np.zeros((64, 128), np.float32)], 0).astype(np.float32)
    shared = {
        "pvec1": pvec1, "pvec2": pvec2, "b_ada": f("b_ada")[0].reshape(1, -1), "fgain": f("final_norm_gain").reshape(1, -1),
        "w_ada": f("w_ada")[0], "w_in": f("w_in")[0], "pool_w": f("pool_w")[0].reshape(2048, 512),
        "wbp": f("w_branch_pool")[0], "wbr": f("w_branch_ret")[0], "wout": f("w_out")[0], "wq": f("peer_w_query")[0],
        "keys": f("peer_sub_keys")[0].reshape(2048, 128), "peer_u": f("peer_u")[0], "peer_v": f("peer_v")[0],
    }
    in_maps = []
    for core in range(NCORES):
        s0 = core * TPC
        npre = NPRE * BLK
        xp = np.zeros((npre, D), np.float32)
        pp = np.zeros((1, npre), np.int32)
        lo = s0 - npre
        if lo < 0:
            if s0 > 0:
                xp[-s0:] = x[0:s0]
                pp[0, -s0:] = pos[0:s0]
        else:
            xp[:] = x[lo:s0]
            pp[0, :] = pos[lo:s0]
        m = dict(shared)
        m["x_own"] = np.ascontiguousarray(x[s0:s0 + TPC])
        m["x_pre"] = xp
        m["pos_own"] = np.ascontiguousarray(pos[s0:s0 + TPC]).reshape(1, TPC)
        m["pos_pre"] = pp
        m["consts"] = _consts(core, BLK, NT, NPRE, lay, NCONST, TPC)
        in_maps.append(m)
    res = run_bass_kernel_spmd(nc, in_maps, core_ids=list(range(NCORES)), trace=trace)
    outs = [np.asarray(r["out"]) for r in res.results]
    return np.concatenate(outs, 0).reshape(1, SEQ, D).astype(np.float32), res


def kernel(**inputs):
    out, _ = _run(inputs, 16384, 1024)
    return out
```
